# Optimizing a Trainium2 kernel written in Bass

```python
import jax, jax.numpy as jnp
from jax import lax
import numpy as np

D_MODEL = 2048
BATCH = 8
SEQ = 2048
DEPTH = 1

GLA_HEADS = 4
GLA_DK = 128
GLA_DV = 256
GLA_GATE_RANK = 16
GLA_GATE_NORM = 16.0
GLA_CHUNK = 64
SWA_Q_HEADS = 16
SWA_KV_HEADS = 4
SWA_GROUP = SWA_Q_HEADS // SWA_KV_HEADS
SWA_HEAD_DIM = 64
SWA_WINDOW = 128
SWA_BLOCK = 128
N_GROUPS = 4
EXPERTS_PER_GROUP = 16
N_EXPERTS = N_GROUPS * EXPERTS_PER_GROUP
EXPERT_TOP_K = 2
D_EXPERT = 256
RMS_EPS = 1e-6

GLA_QK_W = GLA_HEADS * GLA_DK
GLA_V_W = GLA_HEADS * GLA_DV
SWA_Q_W = SWA_Q_HEADS * SWA_HEAD_DIM
SWA_KV_W = SWA_KV_HEADS * SWA_HEAD_DIM
MIX_WIDTH = GLA_V_W + SWA_Q_W
IN_SPLITS = (GLA_QK_W, GLA_QK_W, GLA_V_W, GLA_V_W, GLA_GATE_RANK, SWA_Q_W, SWA_KV_W, SWA_KV_W)
IN_WIDTH = GLA_QK_W * 2 + GLA_V_W * 2 + GLA_GATE_RANK + SWA_Q_W + SWA_KV_W * 2

kernel_name = "hymba_gla_swa_sink_alibi_hmoe"


def rms_norm(x, g):
    xf = x.astype(jnp.float32)
    y = xf * lax.rsqrt(jnp.mean(xf * xf, axis=-1, keepdims=True) + RMS_EPS)
    return (y * g.astype(jnp.float32)).astype(x.dtype)


def split_columns(p):
    outs, start = [], 0
    for w in IN_SPLITS:
        outs.append(p[..., start:start + w])
        start += w
    return outs


def alibi_slopes(n_heads):
    return 2.0 ** (-8.0 * jnp.arange(1, n_heads + 1, dtype=jnp.float32) / n_heads)


def gla_mixer(q, k, v, r, gate_lr, w_gk_up, b_gk, norm_g):
    B, T = q.shape[0], q.shape[1]
    C = GLA_CHUNK
    N = T // C
    f32 = jnp.float32
    qf = q.astype(f32).reshape(B, N, C, GLA_HEADS, GLA_DK) * (GLA_DK ** -0.5)
    kf = k.astype(f32).reshape(B, N, C, GLA_HEADS, GLA_DK)
    vf = v.astype(f32).reshape(B, N, C, GLA_HEADS, GLA_DV)
    log_a = jax.nn.log_sigmoid((gate_lr @ w_gk_up + b_gk).astype(f32)) / GLA_GATE_NORM
    log_a = log_a.reshape(B, N, C, GLA_HEADS, GLA_DK)
    bcum = jnp.cumsum(log_a, axis=2)
    b_mid = bcum[:, :, C // 2 - 1:C // 2]
    q_in = qf * jnp.exp(bcum - b_mid)
    k_in = kf * jnp.exp(b_mid - bcum)
    A = jnp.einsum('bnihd,bnjhd->bnhij', q_in, k_in)
    causal = jnp.tril(jnp.ones((C, C), dtype=bool))
    A = jnp.where(causal, A, 0.0)
    o_intra = jnp.einsum('bnhij,bnjhv->bnihv', A, vf)
    b_last = bcum[:, :, -1]
    k_dec = kf * jnp.exp(b_last[:, :, None] - bcum)
    kv = jnp.einsum('bnjhd,bnjhv->bnhdv', k_dec, vf)
    decay = jnp.exp(b_last)

    def step(S, inp):
        kv_n, dec_n = inp
        return dec_n[..., None] * S + kv_n, S

    S0 = jnp.zeros((B, GLA_HEADS, GLA_DK, GLA_DV), f32)
    _, S_prev = lax.scan(step, S0, (jnp.moveaxis(kv, 1, 0), jnp.moveaxis(decay, 1, 0)))
    o_inter = jnp.einsum('bnihd,nbhdv->bnihv', qf * jnp.exp(bcum), S_prev)
    o = (o_intra + o_inter).reshape(B, T, GLA_HEADS, GLA_DV)
    o = o * lax.rsqrt(jnp.mean(o * o, axis=-1, keepdims=True) + RMS_EPS) * norm_g.astype(f32)
    o = o.reshape(B, T, GLA_V_W) * jax.nn.silu(r.astype(f32))
    return o.astype(q.dtype)


def swa_mixer(q, k, v, sinks):
    B, T = q.shape[0], q.shape[1]
    Q = SWA_BLOCK
    NB = T // Q
    f32 = jnp.float32
    qb = q.astype(f32).reshape(B, NB, Q, SWA_KV_HEADS, SWA_GROUP, SWA_HEAD_DIM) * (SWA_HEAD_DIM ** -0.5)

    def band(t):
        t = t.astype(f32).reshape(B, T, SWA_KV_HEADS, SWA_HEAD_DIM)
        tp = jnp.pad(t, ((0, 0), (Q, 0), (0, 0), (0, 0))).reshape(B, NB + 1, Q, SWA_KV_HEADS, SWA_HEAD_DIM)
        return jnp.concatenate([tp[:, :-1], tp[:, 1:]], axis=2)

    kb, vb = band(k), band(v)
    s = jnp.einsum('bnqhgd,bnkhd->bhgnqk', qb, kb)
    rel = jnp.arange(Q)[:, None] + Q - jnp.arange(2 * Q)[None, :]
    key_pos = jnp.arange(NB)[:, None] * Q - Q + jnp.arange(2 * Q)[None, :]
    valid = ((rel >= 0) & (rel < SWA_WINDOW))[None] & (key_pos >= 0)[:, None, :]
    slopes = alibi_slopes(SWA_Q_HEADS).reshape(SWA_KV_HEADS, SWA_GROUP)
    s = s - slopes[:, :, None, None, None] * rel.astype(f32)
    s = jnp.where(valid, s, -jnp.inf)
    sink = sinks.astype(f32).reshape(SWA_KV_HEADS, SWA_GROUP)[:, :, None, None, None]
    m = jnp.maximum(jnp.max(s, axis=-1, keepdims=True), sink)
    p = jnp.exp(s - m)
    p = p / (jnp.sum(p, axis=-1, keepdims=True) + jnp.exp(sink - m))
    o = jnp.einsum('bhgnqk,bnkhd->bnqhgd', p, vb)
    return o.reshape(B, T, SWA_Q_W).astype(q.dtype)


def hier_moe(x, w_group, b_group, w_router, b_router, w_gate, w_up, w_down):
    B, T, D = x.shape
    f32 = jnp.float32
    xt = x.reshape(B * T, D)
    g_logits = (xt @ w_group + b_group).astype(f32)
    g_prob = jax.nn.softmax(g_logits, axis=-1)
    g_idx = jnp.argmax(g_logits, axis=-1)
    g_p = jnp.take_along_axis(g_prob, g_idx[:, None], axis=-1)[:, 0]
    e_logits = (xt @ w_router + b_router).astype(f32).reshape(-1, N_GROUPS, EXPERTS_PER_GROUP)
    sel = jnp.take_along_axis(e_logits, g_idx[:, None, None], axis=1)[:, 0]
    e_prob = jax.nn.softmax(sel, axis=-1)
    top_w, top_i = lax.top_k(e_prob, EXPERT_TOP_K)
    top_w = top_w / jnp.sum(top_w, axis=-1, keepdims=True)
    within = jnp.sum(jax.nn.one_hot(top_i, EXPERTS_PER_GROUP, dtype=f32) * top_w[..., None], axis=1)
    combine = (jax.nn.one_hot(g_idx, N_GROUPS, dtype=f32)[:, :, None]
               * (g_p[:, None] * within)[:, None, :]).astype(x.dtype)
    wg = w_gate.reshape(N_GROUPS, EXPERTS_PER_GROUP, D, D_EXPERT)
    wu = w_up.reshape(N_GROUPS, EXPERTS_PER_GROUP, D, D_EXPERT)
    wd = w_down.reshape(N_GROUPS, EXPERTS_PER_GROUP, D_EXPERT, D)
    out = jnp.zeros_like(xt)
    for g in range(N_GROUPS):
        h = jax.nn.silu(jnp.einsum('td,edf->tef', xt, wg[g])) * jnp.einsum('td,edf->tef', xt, wu[g])
        out = out + jnp.einsum('tef,efd->td', h * combine[:, g, :, None], wd[g])
    return out.reshape(B, T, D)


def setup_inputs(seed: int = 0) -> dict:
    key = jax.random.key(seed)
    ks = jax.random.split(key, 20)
    L, D = DEPTH, D_MODEL
    nrm = lambda k, shape, scale: jax.random.normal(k, shape, jnp.float32) * scale
    return {
        "x": nrm(ks[0], (BATCH, SEQ, D), 1.0),
        "norm_mix_g": 1.0 + nrm(ks[1], (L, D), 0.02),
        "w_in": nrm(ks[2], (L, D, IN_WIDTH), D ** -0.5),
        "w_gk_up": nrm(ks[3], (L, GLA_GATE_RANK, GLA_QK_W), GLA_GATE_RANK ** -0.5),
        "b_gk": nrm(ks[4], (L, GLA_QK_W), 0.1) + 2.0,
        "gla_norm_g": 1.0 + nrm(ks[5], (L, GLA_HEADS, GLA_DV), 0.02),
        "swa_sinks": nrm(ks[6], (L, SWA_Q_HEADS), 0.5),
        "w_out": nrm(ks[7], (L, MIX_WIDTH, D), MIX_WIDTH ** -0.5),
        "norm_ffn_g": 1.0 + nrm(ks[8], (L, D), 0.02),
        "w_group": nrm(ks[9], (L, D, N_GROUPS), D ** -0.5),
        "b_group": nrm(ks[10], (L, N_GROUPS), 0.01),
        "w_router": nrm(ks[11], (L, D, N_EXPERTS), D ** -0.5),
        "b_router": nrm(ks[12], (L, N_EXPERTS), 0.01),
        "w_gate": nrm(ks[13], (L, N_EXPERTS, D, D_EXPERT), D ** -0.5),
        "w_up": nrm(ks[14], (L, N_EXPERTS, D, D_EXPERT), D ** -0.5),
        "w_down": nrm(ks[15], (L, N_EXPERTS, D_EXPERT, D), D_EXPERT ** -0.5),
        "norm_final_g": 1.0 + nrm(ks[16], (D,), 0.02),
    }


def reference(x, norm_mix_g, w_in, w_gk_up, b_gk, gla_norm_g, swa_sinks, w_out,
              norm_ffn_g, w_group, b_group, w_router, b_router, w_gate, w_up, w_down,
              norm_final_g):
    h = x
    for l in range(DEPTH):
        xn = rms_norm(h, norm_mix_g[l])
        proj = xn @ w_in[l]
        gq, gk, gv, gr, glr, sq, sk, sv = split_columns(proj)
        o_gla = gla_mixer(gq, gk, gv, gr, glr, w_gk_up[l], b_gk[l], gla_norm_g[l])
        o_swa = swa_mixer(sq, sk, sv, swa_sinks[l])
        mixed = jnp.concatenate([o_gla, o_swa], axis=-1)
        h = h + mixed @ w_out[l]
        hn = rms_norm(h, norm_ffn_g[l])
        h = h + hier_moe(hn, w_group[l], b_group[l], w_router[l], b_router[l],
                         w_gate[l], w_up[l], w_down[l])
    return rms_norm(h, norm_final_g)
```

```python
import numpy as np
import ml_dtypes
import concourse.bass as bass
import concourse.mybir as mybir
from concourse.bass_utils import run_bass_kernel_spmd

F32 = mybir.dt.float32
F32R = mybir.dt.float32r
BF16 = mybir.dt.bfloat16
AF = mybir.ActivationFunctionType
ALU = mybir.AluOpType
AX = mybir.AxisListType

D = 2048
T = 2048
TT = 256
NTILE = T // TT
NE = 64
DFF = 256
EPS = 1e-6
INW = 4624
SLOPES = [2.0 ** (-8.0 * (h + 1) / 16.0) for h in range(16)]
NDS = 10
NT_RUN = NTILE
NE_RUN = NE
HALVES = 2
CAP = 128
NSLOT = NE * CAP
BIG = 1.0e6
STOP = None
SKIP_GLA = False
G_ORDER = (0, 1, 2, 3)


class _Rec:
    def __getattr__(self, name):
        return lambda *a, **k: (name, a, k)


_REC = _Rec()


class Sch:
    ENG = ("pe", "act", "dve", "pool", "sp")

    def __init__(self, nc, sem):
        self.nc = nc
        self.sem = sem
        self.prog = {k: [] for k in self.ENG}
        self.cnt = {k: 0 for k in self.ENG}
        self.seen = {k: {} for k in self.ENG}
        self.lastw = {}
        self.readers = {}
        self.dpool = {"sp": ["dsp%d" % i for i in range(NDS)], "pool": ["dpl%d" % i for i in range(NDS)]}
        self.dval = {n: 0 for q in self.dpool for n in self.dpool[q]}
        self.dnext = {"sp": 0, "pool": 0}

    def _deps(self, eng, reads, writes):
        toks = []
        for r in reads:
            t = self.lastw.get(r)
            if t is not None:
                toks.append(t)
        for w in writes:
            t = self.lastw.get(w)
            if t is not None:
                toks.append(t)
            toks.extend(self.readers.get(w, {}).items())
        waits = []
        for (s, v) in toks:
            if s == eng and eng == "pe":
                continue
            if self.seen[eng].get(s, 0) >= v:
                continue
            self.seen[eng][s] = v
            waits.append((s, v))
        return waits

    def _update(self, tok, reads, writes):
        for w in writes:
            self.lastw[w] = tok
            self.readers[w] = {}
        for r in reads:
            d = self.readers.setdefault(r, {})
            if d.get(tok[0], 0) < tok[1]:
                d[tok[0]] = tok[1]

    def mark(self, label):
        if STOP is not None and label == STOP:
            self.dead = True

    def op(self, eng, fn, reads=(), writes=(), signal=True):
        if getattr(self, "dead", False):
            return
        waits = self._deps(eng, reads, writes)
        if signal:
            self.cnt[eng] += 1
            tok = (eng, self.cnt[eng])
        else:
            tok = (eng, self.cnt[eng] + 1)
        self.prog[eng].append((waits, fn(_REC), signal, None))
        self._update(tok, reads, writes)

    def barrier(self):
        for e in self.ENG:
            waits = []
            for o in ("pe", "act", "dve"):
                if o != e and self.cnt[o] > self.seen[e].get(o, 0):
                    waits.append((o, self.cnt[o]))
                    self.seen[e][o] = self.cnt[o]
            for nm, v in self.dval.items():
                if v > self.seen[e].get(nm, 0):
                    waits.append((nm, v))
                    self.seen[e][nm] = v
            self.prog[e].append((waits, None, False, None))

    def dma(self, q, out, in_, reads=(), writes=(), out_off=None, in_off=None, bound="slot"):
        if getattr(self, "dead", False):
            return
        waits = self._deps(q, reads, writes)
        names = self.dpool[q]
        nm = names[self.dnext[q]]
        self.dnext[q] = (self.dnext[q] + 1) % len(names)
        prev = self.dval[nm]
        if prev > 0 and self.seen[q].get(nm, 0) < prev:
            waits.append((nm, prev))
            self.seen[q][nm] = prev
        self.dval[nm] = prev + 16
        tok = (nm, prev + 16)
        if out_off is None and in_off is None:
            self.prog[q].append((waits, ("dma_start", (), dict(out=out, in_=in_)), False, nm))
        else:
            oo = None if out_off is None else bass.IndirectOffsetOnAxis(ap=out_off, axis=0)
            io = None if in_off is None else bass.IndirectOffsetOnAxis(ap=in_off, axis=0)
            self.prog[q].append((waits, ("indirect_dma_start", (), dict(out=out, out_offset=oo, in_=in_, in_offset=io,
                                                                          bounds_check=self.bc_reg[bound], oob_is_err=False)), False, nm))
        self._update(tok, reads, writes)

    def finish(self):
        waits = []
        for nm, v in self.dval.items():
            if v > 0 and self.seen["sp"].get(nm, 0) < v:
                waits.append((nm, v))
        for e in ("pe", "act", "dve"):
            if self.cnt[e] > 0:
                waits.append((e, self.cnt[e]))
        self.prog["sp"].append((waits, None, False, None))

    def emit(self, eng, E):
        for waits, fn, signal, dnm in self.prog[eng]:
            for s, v in waits:
                E.wait_ge(self.sem[s], v)
            if fn is None:
                continue
            ins = getattr(E, fn[0])(*fn[1], **fn[2])
            if signal:
                ins.then_inc(self.sem[eng], 1)
            if dnm is not None:
                ins.then_inc(self.sem[dnm], 16)


def build_nc():
    nc = bass.Bass("TRN2", target_bir_lowering=False)
    dt_ = nc.dram_tensor
    x = dt_("x", [T, D], F32, kind="ExternalInput").ap()
    w_in_g = dt_("w_in_g", [18, D, 256], F32R, kind="ExternalInput").ap()
    w_glr = dt_("w_glr", [D, 16], F32R, kind="ExternalInput").ap()
    w_out_g = dt_("w_out_g", [8, D, 256], F32R, kind="ExternalInput").ap()
    w_gate = dt_("w_gate", [NE, D, DFF], F32R, kind="ExternalInput").ap()
    w_up = dt_("w_up", [NE, D, DFF], F32R, kind="ExternalInput").ap()
    w_down = dt_("w_down", [NE, DFF, D], F32R, kind="ExternalInput").ap()
    w_rt = dt_("w_rt", [D, 68], F32, kind="ExternalInput").ap()
    g_mix = dt_("g_mix", [D], F32, kind="ExternalInput").ap()
    g_ffn = dt_("g_ffn", [D], F32, kind="ExternalInput").ap()
    gfin_d = dt_("gfin_bc", [128, D], F32, kind="ExternalInput").ap()
    gnorm_d = dt_("gnorm_bc", [128, 1024], F32, kind="ExternalInput").ap()
    sinks_d = dt_("sinks_bc", [128, 16], F32, kind="ExternalInput").ap()
    bcat_d = dt_("bcat_bc", [128, 68], F32, kind="ExternalInput").ap()
    wgk_d = dt_("wgk_aug", [17, 512], F32, kind="ExternalInput").ap()
    ident_d = dt_("ident", [128, 128], F32, kind="ExternalInput").ap()
    tri_d = dt_("tri", [128, 128], F32, kind="ExternalInput").ap()
    mask4_d = dt_("mask4", [128, 512], F32, kind="ExternalInput").ap()
    relm_d = dt_("relm", [128, 512], F32, kind="ExternalInput").ap()
    rconst_d = dt_("rconst", [128, 192], F32, kind="ExternalInput").ap()
    triu_d = dt_("triu", [128, 128], F32, kind="ExternalInput").ap()
    h1_d = dt_("h1_scr", [T, D], F32, kind="Internal").ap()
    tokid_d = dt_("tokid", [128, 16], mybir.dt.int32, kind="ExternalInput").ap()
    tabinit_d = dt_("tabinit", [NSLOT, 1], mybir.dt.int32, kind="ExternalInput").ap()
    tab_d = dt_("tab_scr", [NSLOT, 1], mybir.dt.int32, kind="Internal").ap()
    Ys_d = dt_("ys_scr", [NSLOT, D], F32, kind="Internal").ap()
    y = dt_("y", [T, D], F32, kind="ExternalOutput").ap()

    GROUP_C0 = [c * 256 for c in range(12)] + [3088, 3344, 3600, 3856, 4112, 4368]
    w_in_grp = {c0: w_in_g[gi].rearrange("(p k) c -> p k c", k=16) for gi, c0 in enumerate(GROUP_C0)}
    w_out_grp = [w_out_g[gi].rearrange("(p k) c -> p k c", k=16) for gi in range(8)]

    import contextlib
    es = contextlib.ExitStack()
    with es:
        sem = {}
        for nm in list(Sch.ENG) + ["dsp%d" % i for i in range(NDS)] + ["dpl%d" % i for i in range(NDS)]:
            sem[nm] = es.enter_context(nc.semaphore(nm))
        S = Sch(nc, sem)
        S.bc_reg = {"slot": nc.alloc_register(mybir.EngineType.Pool, "bc_slot"), "tok": nc.alloc_register(mybir.EngineType.Pool, "bc_tok")}
        S.prog["pool"].append(([], ("reg_mov", (S.bc_reg["slot"], NSLOT - 1), {}), False, None))
        S.prog["pool"].append(([], ("reg_mov", (S.bc_reg["tok"], T - 1), {}), False, None))

        def sb(name, shape, dtype):
            return es.enter_context(nc.sbuf_tensor("s_" + name, shape, dtype))

        PS = [es.enter_context(nc.psum_tensor("ps%d" % i, [128, 512], F32)) for i in range(8)]

        ridx = sb("ridx", [128, 16, 2], mybir.dt.int32)
        wts = sb("wts", [128, 16, 2], F32)
        tokE = sb("tokE", [128, NE], mybir.dt.int32)
        tokid = sb("tokid", [128, 16], mybir.dt.int32)
        Atot = sb("Atot", [128, NE], F32)
        rconst = sb("rconst", [128, 192], F32)
        triu = sb("triu", [128, 128], F32)
        ones = sb("ones", [128, 128], F32)
        gffnc = sb("gffnc", [128, 16], F32)
        ident = sb("ident", [128, 128], F32)
        identb = sb("identb", [128, 128], BF16)
        small = sb("small", [128, 128], F32)
        SS, RSTD, OSS, ORSTD, MX, NEGM, RSUM, SKA, ESK, RDEN = (slice(0, 2), slice(2, 4), slice(4, 8), slice(8, 12),
                                                                 slice(12, 16), slice(16, 20), slice(20, 24),
                                                                 slice(24, 28), slice(28, 32), slice(32, 36))
        rt = sb("rt", [128, 640], F32)

        S.dma("pool", ident[:], ident_d, writes=["ident"])
        S.dma("pool", rconst[:], rconst_d, writes=["rconst"])
        S.dma("pool", triu[:], triu_d, writes=["triu"])
        S.dma("pool", tokid[:], tokid_d, writes=["tokid"])
        S.dma("pool", tab_d.rearrange("(a b) o -> a (b o)", a=128), tabinit_d.rearrange("(a b) o -> a (b o)", a=128), writes=["tab"])
        S.op("dve", lambda E: E.memset(ones[:], 1.0), writes=["ones"])
        S.op("dve", lambda E: E.memset(Atot[:], 0.0), writes=["Atot"])
        S.op("dve", lambda E: E.tensor_copy(out=identb[:], in_=ident[:]), reads=["ident"], writes=["identb"])

        def pow_rstd(dst, src, n, inv, rk, wk):
            S.op("act", lambda E: E.activation(out=dst, in_=src, func=AF.Ln, scale=inv, bias=EPS), reads=rk, writes=wk)
            S.op("act", lambda E: E.activation(out=dst, in_=dst, func=AF.Exp, scale=-0.5), reads=wk, writes=wk)

        with contextlib.ExitStack() as ea:
            def sa(name, shape, dtype):
                return ea.enter_context(nc.sbuf_tensor("a_" + name, shape, dtype))
            xt = [sa("xt%d" % i, [128, 2, D], F32) for i in range(2)]
            xT = sa("xT", [128, 16, TT], F32R)
            W = [sa("W%d" % i, [128, 16, 256], F32R) for i in range(3)]
            tok = sa("tok", [128, D], F32)
            qT = sa("qT", [128, 4, TT], F32)
            kT = sa("kT", [128, 4, TT], F32)
            vtok = sa("vtok", [128, 2, 1024], BF16)
            rs = sa("rs", [128, 2, 1024], BF16)
            sqT = sa("sqT", [128, 8, TT], BF16)
            skT = sa("skT", [128, 4, 128 + TT], BF16)
            svt = sa("svt", [128, 3, 256], BF16)
            ktok = sa("ktok", [128, 2, 256], BF16)
            ktoks = sa("ktoks", [128, 2, 256], BF16)
            glrT = sa("glrT", [32, TT], F32)
            wglr = sa("wglr", [128, 16, 16], F32R)
            wgk = sa("wgk", [32, 512], F32)
            wr = sa("wr", [128, 16, 68], F32)
            gmixc = sa("gmixc", [128, 16], F32)
            gnorm = sa("gnorm", [128, 1024], F32)
            sinks = sa("sinks", [128, 16], F32)
            bcat = sa("bcat", [128, 68], F32)
            tri = sa("tri", [128, 128], F32)
            mask4 = sa("mask4", [128, 512], F32)
            relm = sa("relm", [128, 512], F32)
            la = sa("la", [128, 512], F32)
            la2 = sa("la2", [128, 512], F32)
            bcs = sa("bcs", [128, 512], F32)
            d1 = sa("d1", [128, 512], F32)
            d4 = sa("d4", [128, 512], F32)
            E1 = sa("E1", [128, 512], F32)
            E2 = sa("E2", [128, 512], F32)
            E3 = sa("E3", [128, 512], F32)
            E4 = sa("E4", [128, 512], F32)
            dec = sa("dec", [128, 8], F32)
            qin = sa("qin", [128, 4, 128], BF16)
            qdec0 = sa("qdec0", [128, 4, 128], BF16)
            qdec1 = sa("qdec1", [128, 4, 128], BF16)
            kin = sa("kin", [128, 4, 128], BF16)
            kdT = sa("kdT", [128, 4, 128], BF16)
            kdtok = sa("kdtok", [128, 4, 128], BF16)
            ATs = sa("ATs", [128, 4, 128], BF16)
            Sst = sa("Sst", [128, 1024], F32)
            Sb = [sa("Sb%d" % i, [128, 1024], BF16) for i in range(2)]
            on = sa("on", [128, 1024], F32)
            junk = on[:].bitcast(BF16)
            ssbs = [sa("ssb%d" % i, [128, 4, 256], F32) for i in range(2)]
            pbs = [sa("pb%d" % i, [128, 4, 256], BF16) for i in range(2)]
            pTs = sa("pTs", [128, 8, 128], BF16)
            lg = sa("lg", [128, 68], F32)

            S.dma("sp", xt[0][:], x[0:TT, :].rearrange("(s p) d -> p s d", p=128), writes=["xt0"])
            S.dma("sp", gmixc[:], g_mix.rearrange("(p k) -> p k", k=16), writes=["gmixc"])
            S.dma("sp", tri[:], tri_d, writes=["tri"])
            S.dma("sp", mask4[:], mask4_d, writes=["mask4"])
            S.dma("sp", relm[:], relm_d, writes=["relm"])
            S.dma("sp", gnorm[:], gnorm_d, writes=["gnorm"])
            S.dma("sp", sinks[:], sinks_d, writes=["sinks"])
            S.dma("sp", bcat[:], bcat_d, writes=["bcat"])
            S.dma("sp", wgk[0:17, :], wgk_d, writes=["wgk"])
            S.dma("sp", wr[:], w_rt.rearrange("(p k) c -> p k c", k=16), writes=["wr"])
            S.dma("sp", gffnc[:], g_ffn.rearrange("(p k) -> p k", k=16), writes=["gffnc"])
            S.dma("pool", wglr[:], w_glr.rearrange("(p k) c -> p k c", k=16), writes=["wglr"])
            S.op("dve", lambda E: E.memset(glrT[:], 1.0), writes=["glrT"])
            S.op("dve", lambda E: E.memset(Sst[:], 0.0), writes=["Sst"])
            S.op("dve", lambda E: E.memset(qdec0[:], 0.0), writes=["qdec"])
            S.op("dve", lambda E: E.memset(qdec1[:], 0.0), writes=["qdec"])
            S.op("dve", lambda E: E.memset(Sb[0][:], 0.0), writes=["Sb0"])
            S.op("dve", lambda E: E.memset(skT[:], 0.0), writes=["skT"])
            S.op("dve", lambda E: E.memset(svt[:], 0.0), writes=["svt"])

            wstate = {"n": 0}

            def wload(src):
                i = wstate["n"] % 3
                wstate["n"] += 1
                S.dma("pool", W[i][:], src, writes=["W%d" % i])
                return i

            mmstate = {"n": 0}

            def mmslot():
                i = mmstate["n"] % 2
                mmstate["n"] += 1
                return PS[i][:, 0:256], "p%d" % i

            trstate = {"n": 0}

            def trslot():
                i = trstate["n"] % 2
                trstate["n"] += 1
                return PS[2 + i], "p%d" % (2 + i)

            evstate = {"n": 0}

            def ev_eng():
                evstate["n"] += 1
                return "act" if evstate["n"] % 2 else "dve"

            def norm_T(src_tile, src_key, gcol, gkey, dst_key):
                for sub in range(2):
                    src = src_tile[:, sub, :]
                    S.op("act", lambda E, src=src, sub=sub: E.activation(out=junk, in_=src, func=AF.Square,
                                                                         accum_out=small[:, sub:sub + 1]),
                         reads=[src_key], writes=["on", "ss"])
                pow_rstd(small[:, RSTD], small[:, SS], 2, 1.0 / D, ["ss"], ["rstd"])
                for sub in range(2):
                    src = src_tile[:, sub, :]
                    S.op("act", lambda E, src=src, sub=sub: E.activation(out=tok[:], in_=src, func=AF.Copy,
                                                                         scale=small[:, 2 + sub:3 + sub]),
                         reads=[src_key, "rstd"], writes=["tok", "tokg", "toks"])
                    transpose_tok(sub, gcol, gkey, dst_key)

            def transpose_tok(sub, gcol, gkey, dst_key):
                tokv = tok[:].rearrange("p (q k) -> p k q", k=16)
                for g4 in range(4):
                    ps, pk = trslot()
                    for i in range(4):
                        kc = g4 * 4 + i
                        S.op("pe", lambda E, ps=ps, i=i, kc=kc: E.transpose(out=ps[:, i * 128:(i + 1) * 128], in_=tokv[:, kc, :],
                                                                            identity=ident[:]),
                             reads=["tok", "tokg", "toks", "ident"], writes=[pk], signal=(i == 3))
                    dst = xT[:, g4 * 4:g4 * 4 + 4, sub * 128:(sub + 1) * 128]
                    psv = ps[:].rearrange("p (a b) -> p a b", a=4)
                    if gcol is not None:
                        gb = gcol[:, g4 * 4:g4 * 4 + 4].unsqueeze(2).to_broadcast([128, 4, 128])
                        S.op("dve", lambda E, dst=dst, psv=psv, gb=gb: E.tensor_tensor(out=dst, in0=psv, in1=gb, op=ALU.mult),
                             reads=[pk, gkey], writes=[dst_key])
                    else:
                        e = ev_eng()
                        if e == "act":
                            S.op("act", lambda E, dst=dst, psv=psv: E.activation(out=dst, in_=psv, func=AF.Copy),
                                 reads=[pk], writes=[dst_key])
                        else:
                            S.op("dve", lambda E, dst=dst, psv=psv: E.tensor_copy(out=dst, in_=psv), reads=[pk], writes=[dst_key])

            def load_x(t):
                b = t % 2
                S.dma("sp", xt[b][:], x[t * TT:(t + 1) * TT, :].rearrange("(s p) d -> p s d", p=128), writes=["xt%d" % b])

            pending_scatter = []

            def flush_scatter():
                while pending_scatter:
                    sg_, k_ = pending_scatter.pop(0)
                    S.dma("pool", tab_d[:, :], tokid[:, sg_:sg_ + 1], reads=["tokid", "ridx"], writes=["tab"], out_off=ridx[:, sg_, k_:k_ + 1])

            S.mark("const")
            for t in range(NT_RUN):
                b = t % 2
                xb = xt[b]
                xk = "xt%d" % b
                if t + 1 < NT_RUN:
                    load_x(t + 1)
                norm_T(xb, xk, gmixc, "gmixc", "xT")

                S.mark("norm1")
                ps, pk = mmslot()
                for kc in range(16):
                    S.op("pe", lambda E, ps=ps, kc=kc: E.matmul(ps[0:16, :], lhsT=wglr[:, kc, :], rhs=xT[:, kc, :],
                                                               start=(kc == 0), stop=(kc == 15)),
                         reads=["wglr", "xT"], writes=[pk], signal=(kc == 15))
                S.op("act", lambda E, ps=ps: E.activation(out=glrT[0:16, :], in_=ps[0:16, :], func=AF.Copy),
                     reads=[pk], writes=["glrT"])

                def fm_group(c0, evac):
                    wi = wload(w_in_grp[c0])
                    for j in range(2):
                        ps, pk = mmslot()
                        for kc in range(16):
                            S.op("pe", lambda E, ps=ps, kc=kc, wi=wi, j=j: E.matmul(ps, lhsT=W[wi][:, kc, j * 128:(j + 1) * 128],
                                                                                 rhs=xT[:, kc, :], start=(kc == 0), stop=(kc == 15)),
                                 reads=["W%d" % wi, "xT"], writes=[pk], signal=(kc == 15))
                        evac(j, ps, pk)

                def tm_group(c0, evac):
                    wi = wload(w_in_grp[c0])
                    for sub in range(2):
                        ps, pk = mmslot()
                        for kc in range(16):
                            S.op("pe", lambda E, ps=ps, kc=kc, wi=wi, sub=sub: E.matmul(ps, lhsT=xT[:, kc, sub * 128:(sub + 1) * 128],
                                                                                     rhs=W[wi][:, kc, :], start=(kc == 0), stop=(kc == 15)),
                                 reads=["W%d" % wi, "xT"], writes=[pk], signal=(kc == 15))
                        evac(sub, ps, pk)

                for gi in range(2):
                    def ev(j, ps, pk, gi=gi):
                        h = gi * 2 + j
                        S.op("act", lambda E: E.activation(out=qT[:, h, :], in_=ps, func=AF.Copy, scale=128.0 ** -0.5),
                             reads=[pk], writes=["qT"])
                    fm_group(gi * 256, ev)
                for gi in range(2):
                    def ev(j, ps, pk, gi=gi):
                        h = gi * 2 + j
                        S.op("dve", lambda E: E.tensor_copy(out=kT[:, h, :], in_=ps), reads=[pk], writes=["kT"])
                    fm_group(512 + gi * 256, ev)
                flush_scatter()
                for gi in range(4):
                    def ev(sub, ps, pk, gi=gi):
                        S.op("dve", lambda E: E.tensor_copy(out=vtok[:, sub, gi * 256:(gi + 1) * 256], in_=ps),
                             reads=[pk], writes=["vtok"])
                    tm_group(1024 + gi * 256, ev)
                for gi in range(4):
                    def ev(sub, ps, pk, gi=gi):
                        S.op("act", lambda E: E.activation(out=rs[:, sub, gi * 256:(gi + 1) * 256], in_=ps, func=AF.Silu),
                             reads=[pk], writes=["rs"])
                    tm_group(2048 + gi * 256, ev)
                for gi in range(4):
                    def ev(j, ps, pk, gi=gi):
                        c = gi * 2 + j
                        S.op("dve", lambda E: E.tensor_scalar(out=sqT[:, c, :], in0=ps, scalar1=0.125, scalar2=None, op0=ALU.mult),
                             reads=[pk], writes=["sqT"])
                    fm_group(3088 + gi * 256, ev)
                def ev(sub, ps, pk):
                    S.op("act", lambda E: E.activation(out=ktok[:, sub, :], in_=ps, func=AF.Copy), reads=[pk], writes=["ktok"])
                    kv4 = ktok[:, sub, :].rearrange("p (a b c) -> p a b c", a=2, b=2)
                    ks4 = ktoks[:, sub, :].rearrange("p (a b c) -> p a b c", a=2, b=2)
                    S.op("dve", lambda E: E.tensor_copy(out=ks4[:, :, 0, :], in_=kv4[:, :, 1, :]), reads=["ktok"], writes=["ktoks"])
                    S.op("dve", lambda E: E.tensor_copy(out=ks4[:, :, 1, :], in_=kv4[:, :, 0, :]), reads=["ktok"], writes=["ktoks"])
                    trp, trk = trslot()
                    trb = trp[:].bitcast(BF16)
                    srcs = [ktok[:, sub, 0:128], ktoks[:, sub, 0:128], ktok[:, sub, 128:256], ktoks[:, sub, 128:256]]
                    for i in range(4):
                        S.op("pe", lambda E, i=i: E.transpose(out=trb[:, i * 128:(i + 1) * 128], in_=srcs[i], identity=identb[:]),
                             reads=["ktok", "ktoks", "identb"], writes=[trk], signal=(i == 3))
                    c0 = 128 + sub * 128
                    tv = trb[:, 0:512].rearrange("p (g q) -> p g q", g=4)
                    S.op("act", lambda E: E.activation(out=skT[0:64, :, c0:c0 + 128], in_=tv[0:64, :, :], func=AF.Copy),
                         reads=[trk], writes=["skT"])
                    tv2 = trb[:, 0:512].rearrange("p (a b q) -> p a b q", a=2, b=2)
                    sk2 = skT[:, :, c0:c0 + 128].rearrange("p (a b) q -> p a b q", a=2)
                    S.op("dve", lambda E: E.tensor_copy(out=sk2[64:128, :, 0, :], in_=tv2[64:128, :, 1, :]), reads=[trk], writes=["skT"])
                    S.op("dve", lambda E: E.tensor_copy(out=sk2[64:128, :, 1, :], in_=tv2[64:128, :, 0, :]), reads=[trk], writes=["skT"])
                tm_group(4112, ev)

                def ev(sub, ps, pk):
                    S.op("dve", lambda E: E.tensor_copy(out=svt[:, 1 + sub, :], in_=ps), reads=[pk], writes=["svt"])
                tm_group(4368, ev)

                S.mark("inproj")
                wo_idx = []
                for dg in range(2):
                    wo_idx.append(wload(w_out_grp[dg]))

                def gla_gen(sub):
                    ts = slice(sub * 128, (sub + 1) * 128)
                    zp = PS[7]
                    S.op("pe", lambda E: E.matmul(zp[:], lhsT=glrT[0:17, ts], rhs=wgk[0:17, :], start=True, stop=True),
                         reads=["glrT", "wgk"], writes=["p7"])
                    S.op("act", lambda E: E.activation(out=la2[:], in_=zp[:], func=AF.Exp, scale=-1.0), reads=["p7"], writes=["la2"])
                    S.op("act", lambda E: E.activation(out=la[:], in_=la2[:], func=AF.Ln, bias=1.0), reads=["la2"], writes=["la"])
                    yield
                    bp = PS[7]
                    for h in range(4):
                        S.op("pe", lambda E, h=h: E.matmul(bp[:, h * 128:(h + 1) * 128], lhsT=la[:, h * 128:(h + 1) * 128], rhs=tri[:],
                                                           start=True, stop=True),
                             reads=["la", "tri"], writes=["p7"], signal=(h == 3))
                    S.op("act", lambda E: E.activation(out=bcs[:], in_=bp[:], func=AF.Copy), reads=["p7"], writes=["bcs"])
                    yield
                    bv = bcs[:].rearrange("p (a b) -> p a b", b=64)
                    d1v = d1[:].rearrange("p (a b) -> p a b", b=64)
                    d4v = d4[:].rearrange("p (a b) -> p a b", b=64)
                    S.op("dve", lambda E: E.tensor_tensor(out=d1v, in0=bv, in1=bv[:, :, 31:32].to_broadcast([128, 8, 64]), op=ALU.subtract),
                         reads=["bcs"], writes=["d1"])
                    S.op("dve", lambda E: E.tensor_tensor(out=d4v, in0=bv, in1=bv[:, :, 63:64].to_broadcast([128, 8, 64]), op=ALU.subtract),
                         reads=["bcs"], writes=["d4"])
                    S.op("act", lambda E: E.activation(out=E1[:], in_=d1[:], func=AF.Exp), reads=["d1"], writes=["E1"])
                    S.op("act", lambda E: E.activation(out=E2[:], in_=d1[:], func=AF.Exp, scale=-1.0), reads=["d1"], writes=["E2"])
                    S.op("act", lambda E: E.activation(out=E3[:], in_=bcs[:], func=AF.Exp), reads=["bcs"], writes=["E3"])
                    S.op("act", lambda E: E.activation(out=E4[:], in_=d4[:], func=AF.Exp, scale=-1.0), reads=["d4"], writes=["E4"])
                    S.op("act", lambda E: E.activation(out=dec[:].unsqueeze(2), in_=bv[:, :, 63:64], func=AF.Exp), reads=["bcs"], writes=["dec"])
                    yield
                    qv = qT[:, :, ts]
                    kv_ = kT[:, :, ts]
                    e4 = lambda a: a[:].rearrange("p (h i) -> p h i", h=4)
                    S.op("dve", lambda E: E.tensor_tensor(out=qin[:], in0=qv, in1=e4(E1), op=ALU.mult), reads=["qT", "E1"], writes=["qin"])
                    S.op("dve", lambda E: E.tensor_tensor(out=kin[:], in0=kv_, in1=e4(E2), op=ALU.mult), reads=["kT", "E2"], writes=["kin"])
                    S.op("dve", lambda E: E.tensor_tensor(out=qdec0[:, :, 0:64], in0=qv[:, :, 0:64], in1=e4(E3)[:, :, 0:64], op=ALU.mult),
                         reads=["qT", "E3"], writes=["qdec"])
                    S.op("dve", lambda E: E.tensor_tensor(out=qdec1[:, :, 64:128], in0=qv[:, :, 64:128], in1=e4(E3)[:, :, 64:128], op=ALU.mult),
                         reads=["qT", "E3"], writes=["qdec"])
                    S.op("dve", lambda E: E.tensor_tensor(out=kdT[:], in0=kv_, in1=e4(E4), op=ALU.mult), reads=["kT", "E4"], writes=["kdT"])
                    yield
                    ap_ = PS[7]
                    for h in range(4):
                        S.op("pe", lambda E, h=h: E.matmul(ap_[:, h * 128:(h + 1) * 128], lhsT=kin[:, h, :], rhs=qin[:, h, :], start=True, stop=True),
                             reads=["kin", "qin"], writes=["p7"], signal=(h == 3))
                    trp, trk = trslot()
                    trb = trp[:].bitcast(BF16)
                    for h in range(4):
                        S.op("pe", lambda E, h=h, trb=trb: E.transpose(out=trb[:, h * 128:(h + 1) * 128], in_=kdT[:, h, :], identity=identb[:]),
                             reads=["kdT", "identb"], writes=[trk], signal=(h == 3))
                    S.op("dve", lambda E: E.tensor_tensor(out=ATs[:].rearrange("p h i -> p (h i)"), in0=ap_[:], in1=mask4[:], op=ALU.mult),
                         reads=["p7", "mask4"], writes=["ATs"])
                    S.op("act", lambda E, trb=trb: E.activation(out=kdtok[:].rearrange("p h i -> p (h i)"), in_=trb[:, 0:512], func=AF.Copy),
                         reads=[trk], writes=["kdtok"])
                    yield

                    def kv_update(c):
                        cs = slice(c * 64, (c + 1) * 64)
                        for hp in range(2):
                            for hh in range(2):
                                h = hp * 2 + hh
                                kp = PS[6][:, hh * 256:hh * 256 + 256]
                                S.op("pe", lambda E, h=h, kp=kp: E.matmul(kp, lhsT=kdtok[cs, h, :], rhs=vtok[cs, sub, h * 256:(h + 1) * 256],
                                                                          start=True, stop=True),
                                     reads=["kdtok", "vtok"], writes=["p6"], signal=(hh == 1))
                            for hh in range(2):
                                h = hp * 2 + hh
                                kp = PS[6][:, hh * 256:hh * 256 + 256]
                                sv_ = Sst[:, h * 256:(h + 1) * 256]
                                S.op("dve", lambda E, h=h, kp=kp, sv_=sv_: E.scalar_tensor_tensor(
                                    out=sv_, in0=sv_, scalar=dec[:, h * 2 + c:h * 2 + c + 1], in1=kp, op0=ALU.mult, op1=ALU.add),
                                     reads=["Sst", "dec", "p6"], writes=["Sst"])
                        tgt = Sb[(c + 1) % 2]
                        S.op("act", lambda E: E.activation(out=tgt[:], in_=Sst[:], func=AF.Copy),
                             reads=["Sst"], writes=["Sb%d" % ((c + 1) % 2)])

                    kv_update(0)
                    yield
                    for h in range(4):
                        op_ = PS[(h // 2)][:, (h % 2) * 256:(h % 2) * 256 + 256]
                        ok_ = "p%d" % (h // 2)
                        S.op("pe", lambda E, h=h, op_=op_: E.matmul(op_, lhsT=ATs[:, h, :], rhs=vtok[:, sub, h * 256:(h + 1) * 256],
                                                                   start=True, stop=False),
                             reads=["ATs", "vtok"], writes=[ok_], signal=False)
                        S.op("pe", lambda E, h=h, op_=op_: E.matmul(op_, lhsT=qdec0[:, h, :], rhs=Sb[0][:, h * 256:(h + 1) * 256],
                                                                   start=False, stop=False),
                             reads=["qdec", "Sb0"], writes=[ok_], signal=False)
                        S.op("pe", lambda E, h=h, op_=op_: E.matmul(op_, lhsT=qdec1[:, h, :], rhs=Sb[1][:, h * 256:(h + 1) * 256],
                                                                   start=False, stop=True),
                             reads=["qdec", "Sb1"], writes=[ok_], signal=True)
                    yield
                    kv_update(1)
                    yield
                    for h in range(4):
                        op_ = PS[(h // 2)][:, (h % 2) * 256:(h % 2) * 256 + 256]
                        S.op("act", lambda E, h=h, op_=op_: E.activation(out=junk[:, 0:256], in_=op_, func=AF.Square,
                                                                       accum_out=small[:, 4 + h:5 + h]),
                             reads=["p%d" % (h // 2)], writes=["on", "oss"])
                    pow_rstd(small[:, ORSTD], small[:, OSS], 4, 1.0 / 256, ["oss"], ["orstd"])
                    for h in range(4):
                        op_ = PS[(h // 2)][:, (h % 2) * 256:(h % 2) * 256 + 256]
                        S.op("dve", lambda E, h=h, op_=op_: E.scalar_tensor_tensor(
                            out=on[:, h * 256:(h + 1) * 256], in0=op_, scalar=small[:, 8 + h:9 + h], in1=gnorm[:, h * 256:(h + 1) * 256],
                            op0=ALU.mult, op1=ALU.mult),
                             reads=["p%d" % (h // 2), "orstd", "gnorm"], writes=["on"])
                    S.op("dve", lambda E: E.tensor_tensor(out=tok[:, 0:1024], in0=on[:], in1=rs[:, sub, :], op=ALU.mult),
                         reads=["on", "rs"], writes=["tokg"])

                def swa_gen(sub, g):
                    ts = slice(sub * 128, (sub + 1) * 128)
                    nbg = t * 2 + sub
                    rl = relm[:, 256:512] if nbg == 0 else relm[:, 0:256]
                    par = g % 2
                    ssb, pb = ssbs[par], pbs[par]
                    so = 64 * par
                    sl = lambda x_: slice(x_.start + so, x_.stop + so)
                    kx = lambda nm: "%s%d" % (nm, par)
                    for j in range(4):
                        h = 4 * g + j
                        c = h // 2
                        base = (h % 2) * 64
                        sp_ = PS[4 + j % 2][:, (j // 2) * 256:(j // 2) * 256 + 256]
                        S.op("pe", lambda E, c=c, base=base, sp_=sp_: E.matmul(
                            sp_, lhsT=sqT[base:base + 64, c, ts], rhs=skT[base:base + 64, g, sub * 128:sub * 128 + 256],
                            start=True, stop=True),
                             reads=["sqT", "skT"], writes=["p%d" % (4 + j % 2)], signal=(j >= 2))
                    for j in range(4):
                        h = 4 * g + j
                        sp_ = PS[4 + j % 2][:, (j // 2) * 256:(j // 2) * 256 + 256]
                        S.op("dve", lambda E, j=j, h=h, sp_=sp_: E.scalar_tensor_tensor(
                            out=ssb[:, j, :], in0=rl, scalar=SLOPES[h], in1=sp_, op0=ALU.mult, op1=ALU.add),
                             reads=["relm", "p%d" % (4 + j % 2)], writes=[kx("ssb")])
                    yield
                    S.op("dve", lambda E: E.tensor_reduce(out=small[:, sl(MX)], in_=ssb[:], axis=AX.X, op=ALU.max),
                         reads=[kx("ssb")], writes=[kx("mx")])
                    S.op("dve", lambda E: E.tensor_tensor(out=small[:, sl(MX)], in0=small[:, sl(MX)], in1=sinks[:, 4 * g:4 * g + 4], op=ALU.max),
                         reads=[kx("mx"), "sinks"], writes=[kx("mx")])
                    S.op("dve", lambda E: E.tensor_scalar(out=small[:, sl(NEGM)], in0=small[:, sl(MX)], scalar1=-1.0, scalar2=None, op0=ALU.mult),
                         reads=[kx("mx")], writes=[kx("negm")])
                    S.op("dve", lambda E: E.tensor_tensor(out=small[:, sl(SKA)], in0=sinks[:, 4 * g:4 * g + 4], in1=small[:, sl(MX)], op=ALU.subtract),
                         reads=[kx("mx"), "sinks"], writes=[kx("ska")])
                    for j in range(4):
                        S.op("act", lambda E, j=j: E.activation(out=pb[:, j, :], in_=ssb[:, j, :], func=AF.Exp,
                                                                bias=small[:, so + 16 + j:so + 17 + j], scale=1.0,
                                                                accum_out=small[:, so + 20 + j:so + 21 + j]),
                             reads=[kx("ssb"), kx("negm")], writes=[kx("pb"), kx("rsum")])
                    S.op("act", lambda E: E.activation(out=small[:, sl(ESK)], in_=small[:, sl(SKA)], func=AF.Exp), reads=[kx("ska")], writes=[kx("esk")])
                    yield
                    S.op("dve", lambda E: E.tensor_tensor(out=small[:, sl(RDEN)], in0=small[:, sl(RSUM)], in1=small[:, sl(ESK)], op=ALU.add),
                         reads=[kx("rsum"), kx("esk")], writes=[kx("rden")])
                    S.op("dve", lambda E: E.reciprocal(out=small[:, sl(RDEN)], in_=small[:, sl(RDEN)]), reads=[kx("rden")], writes=[kx("rden")])
                    trp, trk = trslot()
                    trb = trp[:].bitcast(BF16)
                    for j in range(4):
                        for kb in range(2):
                            i = j * 2 + kb
                            S.op("pe", lambda E, j=j, kb=kb, i=i, trb=trb: E.transpose(
                                out=trb[:, i * 128:(i + 1) * 128], in_=pb[:, j, kb * 128:(kb + 1) * 128], identity=identb[:]),
                                 reads=[kx("pb"), "identb"], writes=[trk], signal=(i == 7))
                    S.op("act", lambda E, trb=trb: E.activation(out=pTs[:].rearrange("p a b -> p (a b)"), in_=trb, func=AF.Copy),
                         reads=[trk], writes=["pTs"])
                    yield
                    op_ = PS[6][:, 0:256]
                    for j in range(4):
                        for kb in range(2):
                            S.op("pe", lambda E, j=j, kb=kb: E.matmul(
                                op_[:, j * 64:(j + 1) * 64], lhsT=pTs[:, j * 2 + kb, :], rhs=svt[:, sub + kb, g * 64:(g + 1) * 64],
                                start=(kb == 0), stop=(kb == 1)),
                                 reads=["pTs", "svt"], writes=["p6"], signal=(j == 3 and kb == 1))
                    S.op("dve", lambda E: E.tensor_tensor(
                        out=tok[:, 1024 + g * 256:1024 + (g + 1) * 256].rearrange("p (a b) -> p a b", a=4),
                        in0=op_.rearrange("p (a b) -> p a b", a=4),
                        in1=small[:, sl(RDEN)].unsqueeze(2).to_broadcast([128, 4, 64]), op=ALU.mult),
                         reads=["p6", kx("rden")], writes=["toks"])
                    yield

                for sub in range(2):
                    gens = {"gla": gla_gen(sub)}
                    for g in range(4):
                        gens[g] = swa_gen(sub, g)
                    live = set(gens.keys())
                    rnd = 0
                    while live:
                        for key in ["gla", 0, 1, 2, 3]:
                            if key not in live:
                                continue
                            if key != "gla" and rnd < 2 * key:
                                continue
                            try:
                                next(gens[key])
                            except StopIteration:
                                live.discard(key)
                        rnd += 1
                    transpose_tok(sub, None, None, "xT")

                S.mark("mixT")
                S.op("dve", lambda E: E.tensor_copy(out=skT[:, :, 0:128], in_=skT[:, :, TT:TT + 128]), reads=["skT"], writes=["skT"])
                S.op("dve", lambda E: E.tensor_copy(out=svt[:, 0, :], in_=svt[:, 2, :]), reads=["svt"], writes=["svt"])

                for dg in range(8):
                    if dg + 2 < 8:
                        wo_idx.append(wload(w_out_grp[dg + 2]))
                    wi = wo_idx[dg]
                    for sub in range(2):
                        ps, pk = mmslot()
                        for c in range(16):
                            S.op("pe", lambda E, ps=ps, c=c, wi=wi, sub=sub: E.matmul(ps, lhsT=xT[:, c, sub * 128:(sub + 1) * 128],
                                                                                   rhs=W[wi][:, c, :], start=(c == 0), stop=(c == 15)),
                                 reads=["W%d" % wi, "xT"], writes=[pk], signal=(c == 15))
                        dst = xb[:, sub, dg * 256:(dg + 1) * 256]
                        S.op("dve", lambda E, ps=ps, dst=dst: E.tensor_tensor(out=dst, in0=ps, in1=dst, op=ALU.add),
                             reads=[pk, xk], writes=[xk])
                S.dma("sp", h1_d[t * TT:(t + 1) * TT, :].rearrange("(s p) d -> p s d", p=128), xb[:], reads=[xk], writes=["h1d"])

                S.mark("outproj")
                norm_T(xb, xk, gffnc, "gffnc", "xT")
                for sub in range(2):
                    sg = t * 2 + sub
                    ps, pk = mmslot()
                    for kc in range(16):
                        S.op("pe", lambda E, ps=ps, kc=kc, sub=sub: E.matmul(ps[:, 0:68], lhsT=xT[:, kc, sub * 128:(sub + 1) * 128].bitcast(F32),
                                                                          rhs=wr[:, kc, :], start=(kc == 0), stop=(kc == 15)),
                             reads=["xT", "wr"], writes=[pk], signal=(kc == 15))
                    S.op("dve", lambda E, ps=ps: E.tensor_tensor(out=lg[:], in0=ps[:, 0:68], in1=bcat[:], op=ALU.add),
                         reads=[pk, "bcat"], writes=["lg"])
                    R = lambda a, b: rt[:, a:b]
                    GM, GOH, GE, GS, SEL, M1, OH1, SEL2, M2, OH2, DL, W1, W2, WI = (R(0, 1), R(4, 8), R(8, 12), R(12, 13), R(16, 32), R(32, 33),
                                                                                 R(48, 64), R(64, 80), R(80, 81), R(96, 112), R(112, 113),
                                                                                 R(113, 114), R(114, 115), R(128, 144))
                    NGM = R(1, 2)
                    dv = lambda fn, r, w: S.op("dve", fn, reads=r, writes=w)
                    dv(lambda E: E.tensor_reduce(out=GM, in_=lg[:, 0:4], axis=AX.X, op=ALU.max), ["lg"], ["rt"])
                    dv(lambda E: E.tensor_scalar(out=GOH, in0=lg[:, 0:4], scalar1=GM, scalar2=None, op0=ALU.is_equal), ["lg", "rt"], ["rt"])
                    dv(lambda E: E.tensor_scalar(out=NGM, in0=GM, scalar1=-1.0, scalar2=None, op0=ALU.mult), ["rt"], ["rt"])
                    S.op("act", lambda E: E.activation(out=GE, in_=lg[:, 0:4], func=AF.Exp, bias=NGM, scale=1.0, accum_out=GS),
                         reads=["lg", "rt"], writes=["rt"])
                    dv(lambda E: E.reciprocal(out=GS, in_=GS), ["rt"], ["rt"])
                    dv(lambda E: E.tensor_scalar(out=SEL, in0=lg[:, 4:20], scalar1=rt[:, 4:5], scalar2=None, op0=ALU.mult), ["lg", "rt"], ["rt"])
                    for g in range(1, 4):
                        dv(lambda E, g=g: E.scalar_tensor_tensor(out=SEL, in0=lg[:, 4 + 16 * g:20 + 16 * g], scalar=rt[:, 4 + g:5 + g], in1=SEL,
                                                                 op0=ALU.mult, op1=ALU.add), ["lg", "rt"], ["rt"])
                    dv(lambda E: E.tensor_reduce(out=M1, in_=SEL, axis=AX.X, op=ALU.max), ["rt"], ["rt"])
                    dv(lambda E: E.tensor_scalar(out=OH1, in0=SEL, scalar1=M1, scalar2=None, op0=ALU.is_equal), ["rt"], ["rt"])
                    dv(lambda E: E.scalar_tensor_tensor(out=SEL2, in0=OH1, scalar=-1e30, in1=SEL, op0=ALU.mult, op1=ALU.add), ["rt"], ["rt"])
                    dv(lambda E: E.tensor_reduce(out=M2, in_=SEL2, axis=AX.X, op=ALU.max), ["rt"], ["rt"])
                    dv(lambda E: E.tensor_scalar(out=OH2, in0=SEL2, scalar1=M2, scalar2=None, op0=ALU.is_equal), ["rt"], ["rt"])
                    dv(lambda E: E.tensor_tensor(out=DL, in0=M2, in1=M1, op=ALU.subtract), ["rt"], ["rt"])
                    S.op("act", lambda E: E.activation(out=W2, in_=DL, func=AF.Exp), reads=["rt"], writes=["rt"])
                    dv(lambda E: E.tensor_scalar(out=W1, in0=W2, scalar1=1.0, scalar2=None, op0=ALU.add), ["rt"], ["rt"])
                    dv(lambda E: E.reciprocal(out=W1, in_=W1), ["rt"], ["rt"])
                    dv(lambda E: E.tensor_tensor(out=W2, in0=W2, in1=W1, op=ALU.mult), ["rt"], ["rt"])
                    dv(lambda E: E.tensor_tensor(out=W1, in0=W1, in1=GS, op=ALU.mult), ["rt"], ["rt"])
                    dv(lambda E: E.tensor_tensor(out=W2, in0=W2, in1=GS, op=ALU.mult), ["rt"], ["rt"])
                    dv(lambda E: E.tensor_scalar(out=WI, in0=OH1, scalar1=W1, scalar2=None, op0=ALU.mult), ["rt"], ["rt"])
                    dv(lambda E: E.scalar_tensor_tensor(out=WI, in0=OH2, scalar=W2, in1=WI, op0=ALU.mult, op1=ALU.add), ["rt"], ["rt"])
                    A1, A2, AA, TMP, OVF, P1, RF, OKK = (R(192, 256), R(256, 320), R(320, 384), R(384, 448), R(448, 512),
                                                         R(512, 576), R(576, 578), R(578, 580))
                    a3 = lambda a: a.rearrange("p (g j) -> p g j", g=4)
                    gb3 = GOH.unsqueeze(2).to_broadcast([128, 4, 16])
                    dv(lambda E: E.tensor_tensor(out=a3(A1), in0=gb3, in1=OH1.unsqueeze(1).to_broadcast([128, 4, 16]), op=ALU.mult),
                       ["rt"], ["rt"])
                    dv(lambda E: E.tensor_tensor(out=a3(A2), in0=gb3, in1=OH2.unsqueeze(1).to_broadcast([128, 4, 16]), op=ALU.mult),
                       ["rt"], ["rt"])
                    dv(lambda E: E.tensor_tensor(out=AA, in0=A1, in1=A2, op=ALU.add), ["rt"], ["rt"])
                    ps, pk = mmslot()
                    S.op("pe", lambda E, ps=ps: E.matmul(ps[:, 0:NE], lhsT=triu[:], rhs=AA, start=True, stop=False),
                         reads=["rt", "triu"], writes=[pk], signal=False)
                    S.op("pe", lambda E, ps=ps: E.matmul(ps[:, 0:NE], lhsT=ones[:], rhs=Atot[:], start=False, stop=True),
                         reads=["Atot", "ones"], writes=[pk], signal=True)
                    dv(lambda E, ps=ps: E.tensor_scalar(out=OVF, in0=ps[:, 0:NE], scalar1=CAP + 0.5, scalar2=BIG, op0=ALU.is_gt, op1=ALU.mult),
                       [pk], ["rt"])
                    dv(lambda E, ps=ps: E.scalar_tensor_tensor(out=TMP, in0=ps[:, 0:NE], scalar=float(NE), in1=rconst[:, 0:NE],
                                                               op0=ALU.mult, op1=ALU.add), [pk, "rconst"], ["rt"])
                    dv(lambda E: E.tensor_tensor(out=TMP, in0=TMP, in1=OVF, op=ALU.add), ["rt"], ["rt"])
                    dv(lambda E: E.tensor_tensor(out=P1, in0=A1, in1=TMP, op=ALU.mult), ["rt"], ["rt"])
                    dv(lambda E: E.tensor_reduce(out=RF[:, 0:1], in_=P1, axis=AX.X, op=ALU.add), ["rt"], ["rt"])
                    dv(lambda E: E.tensor_tensor(out=P1, in0=A2, in1=TMP, op=ALU.mult), ["rt"], ["rt"])
                    dv(lambda E: E.tensor_reduce(out=RF[:, 1:2], in_=P1, axis=AX.X, op=ALU.add), ["rt"], ["rt"])
                    dv(lambda E, sg=sg: E.tensor_copy(out=ridx[:, sg, :], in_=RF), ["rt"], ["ridx"])
                    dv(lambda E: E.tensor_scalar(out=OKK, in0=RF, scalar1=BIG * 0.5, scalar2=None, op0=ALU.is_lt), ["rt"], ["rt"])
                    dv(lambda E, sg=sg: E.tensor_tensor(out=wts[:, sg, :], in0=rt[:, 113:115], in1=OKK, op=ALU.mult), ["rt"], ["wts"])
                    dv(lambda E: E.tensor_tensor(out=Atot[:], in0=Atot[:], in1=AA, op=ALU.add), ["rt", "Atot"], ["Atot"])
                    for k in range(2):
                        pending_scatter.append((sg, k))

            flush_scatter()

        S.barrier()
        with contextlib.ExitStack() as eb:
            def sm(name, shape, dtype):
                return eb.enter_context(nc.sbuf_tensor("b_" + name, shape, dtype))
            Xg = [sm("Xg%d" % i, [128, D], F32) for i in range(2)]
            XT = [sm("XT%d" % i, [128, 16, 128], F32R) for i in range(2)]
            WG = [sm("WG%d" % i, [128, 16, DFF], F32R) for i in range(2)]
            WU = [sm("WU%d" % i, [128, 16, DFF], F32R) for i in range(2)]
            WD = [sm("WD%d" % i, [128, 2, D], F32R) for i in range(2)]
            Yb = [sm("Yb%d" % i, [128, D], F32) for i in range(2)]
            SG = sm("SG", [128, DFF], F32)
            Hh = sm("Hh", [128, DFF], F32)
            HT = sm("HT", [128, 2, 128], F32R)
            junkb = sm("junkb", [128, D], BF16)
            wg_r = [w_gate[e].rearrange("(p k) f -> p k f", k=16) for e in range(NE)]
            wu_r = [w_up[e].rearrange("(p k) f -> p k f", k=16) for e in range(NE)]
            wd_r = [w_down[e].rearrange("(p c) d -> p c d", c=2) for e in range(NE)]
            for i in range(2):
                S.op("dve", lambda E, i=i: E.memset(Xg[i][:], 0.0), writes=["Xg%d" % i])
            S.dma("pool", tokE[:], tab_d.rearrange("(s e) o -> s (e o)", e=NE), reads=["tab"], writes=["tokE"])
            ys_r = Ys_d.rearrange("(s e) d -> e s d", e=NE)

            def b_load(e):
                i = e % 2
                S.dma("pool", Xg[i][:, :], h1_d[:, :], reads=["h1d", "tokE"], writes=["Xg%d" % i], in_off=tokE[:, e:e + 1], bound="tok")
                S.dma("pool", WG[i][:], wg_r[e], writes=["WG%d" % i])
                S.dma("pool", WU[i][:], wu_r[e], writes=["WU%d" % i])
                S.dma("pool", WD[i][:], wd_r[e], writes=["WD%d" % i])

            def b_transpose(e):
                i = e % 2
                S.op("act", lambda E: E.activation(out=junkb[:], in_=Xg[i][:], func=AF.Square, accum_out=small[:, 44 + i:45 + i]),
                     reads=["Xg%d" % i], writes=["junkb", "bss%d" % i])
                pow_rstd(small[:, 46 + i:47 + i], small[:, 44 + i:45 + i], 1, 1.0 / D, ["bss%d" % i], ["brstd%d" % i])
                xv = Xg[i][:].rearrange("p (q k) -> p k q", k=16)
                for g4 in range(4):
                    ps, pk = PS[g4 % 2], "p%d" % (g4 % 2)
                    for j in range(4):
                        kc = g4 * 4 + j
                        S.op("pe", lambda E, ps=ps, j=j, kc=kc: E.transpose(out=ps[:, j * 128:(j + 1) * 128], in_=xv[:, kc, :], identity=ident[:]),
                             reads=["Xg%d" % i, "ident"], writes=[pk], signal=(j == 3))
                    dst = XT[i][:, g4 * 4:g4 * 4 + 4, :]
                    psv = ps[:].rearrange("p (a b) -> p a b", a=4)
                    if g4 % 2 == 0:
                        gb = gffnc[:, g4 * 4:g4 * 4 + 4].unsqueeze(2).to_broadcast([128, 4, 128])
                        S.op("dve", lambda E, dst=dst, psv=psv, gb=gb: E.tensor_tensor(out=dst, in0=psv, in1=gb, op=ALU.mult),
                             reads=[pk, "gffnc"], writes=["XT%d" % i])
                    else:
                        for j in range(4):
                            kc = g4 * 4 + j
                            S.op("act", lambda E, j=j, kc=kc, ps=ps: E.activation(out=XT[i][:, kc, :], in_=ps[:, j * 128:(j + 1) * 128], func=AF.Copy,
                                                                                scale=gffnc[:, kc:kc + 1]),
                                 reads=[pk, "gffnc"], writes=["XT%d" % i])

            def b_gateup(e):
                i = e % 2
                for which, Wt, wk in ((0, WG[i], "WG%d" % i), (1, WU[i], "WU%d" % i)):
                    for kc in range(16):
                        S.op("pe", lambda E, which=which, Wt=Wt, kc=kc: E.matmul(PS[2][:, which * 256:(which + 1) * 256], lhsT=XT[i][:, kc, :],
                                                                               rhs=Wt[:, kc, :], start=(kc == 0), stop=(kc == 15)),
                             reads=["XT%d" % i, wk], writes=["p2"], signal=(kc == 15))
                rstd = small[:, 46 + i:47 + i]
                S.op("act", lambda E: E.activation(out=SG[:], in_=PS[2][:, 0:256], func=AF.Silu, scale=rstd),
                     reads=["p2", "brstd%d" % i], writes=["SG"])
                S.op("dve", lambda E: E.scalar_tensor_tensor(out=Hh[:], in0=PS[2][:, 256:512], scalar=rstd, in1=SG[:], op0=ALU.mult, op1=ALU.mult),
                     reads=["p2", "brstd%d" % i, "SG"], writes=["Hh"])

            def b_down(e):
                i = e % 2
                for fc in range(2):
                    S.op("pe", lambda E, fc=fc: E.transpose(out=PS[3][:, fc * 128:(fc + 1) * 128], in_=Hh[:].rearrange("s (p c) -> s c p", c=2)[:, fc, :], identity=ident[:]),
                         reads=["Hh", "ident"], writes=["p3"], signal=(fc == 1))
                S.op("act", lambda E: E.activation(out=HT[:].rearrange("p a b -> p (a b)"), in_=PS[3][:, 0:256], func=AF.Copy),
                     reads=["p3"], writes=["HT"])
                for dg in range(4):
                    for fc in range(2):
                        S.op("pe", lambda E, dg=dg, fc=fc: E.matmul(PS[4 + dg][:], lhsT=HT[:, fc, :], rhs=WD[i][:, fc, dg * 512:(dg + 1) * 512],
                                                                   start=(fc == 0), stop=(fc == 1)),
                             reads=["HT", "WD%d" % i], writes=["p%d" % (4 + dg)], signal=(fc == 1))
                    dst = Yb[i][:, dg * 512:(dg + 1) * 512]
                    if dg % 2 == 0:
                        S.op("act", lambda E, dg=dg, dst=dst: E.activation(out=dst, in_=PS[4 + dg][:], func=AF.Copy),
                             reads=["p%d" % (4 + dg)], writes=["Yb%d" % i])
                    else:
                        S.op("dve", lambda E, dg=dg, dst=dst: E.tensor_copy(out=dst, in_=PS[4 + dg][:]), reads=["p%d" % (4 + dg)], writes=["Yb%d" % i])
                S.dma("pool", ys_r[e], Yb[i][:, :], reads=["Yb%d" % i], writes=["Ys"])

            b_load(0)
            if NE_RUN > 1:
                b_load(1)
            b_transpose(0)
            for e in range(NE_RUN):
                b_gateup(e)
                if e + 1 < NE_RUN:
                    b_transpose(e + 1)
                b_down(e)
                if e + 2 < NE_RUN:
                    b_load(e + 2)

        S.barrier()
        with contextlib.ExitStack() as ec:
            def sc(name, shape, dtype):
                return ec.enter_context(nc.sbuf_tensor("c_" + name, shape, dtype))
            NB2 = 3
            Y1 = [sc("Y1_%d" % i, [128, D], F32) for i in range(NB2)]
            Y2 = [sc("Y2_%d" % i, [128, D], F32) for i in range(NB2)]
            acc = [sc("acc%d" % i, [128, D], F32) for i in range(NB2)]
            stmp = sc("stmp", [128, 1024], F32)
            gfin = sc("gfin", [128, D], F32)
            S.dma("sp", gfin[:], gfin_d, writes=["gfin"])
            for i in range(NB2):
                S.op("dve", lambda E, i=i: E.memset(Y1[i][:], 0.0), writes=["Y1_%d" % i])
                S.op("dve", lambda E, i=i: E.memset(Y2[i][:], 0.0), writes=["Y2_%d" % i])
            def b2_load(sg):
                i = sg % NB2
                S.dma("pool", Y1[i][:, :], Ys_d[:, :], reads=["Ys", "ridx"], writes=["Y1_%d" % i], in_off=ridx[:, sg, 0:1])
                S.dma("pool", Y2[i][:, :], Ys_d[:, :], reads=["Ys", "ridx"], writes=["Y2_%d" % i], in_off=ridx[:, sg, 1:2])
                S.dma("sp", acc[i][:], h1_d[sg * 128:(sg + 1) * 128, :], reads=["h1d"], writes=["acc%d" % i])

            NSG = 2 * NT_RUN
            for sg in range(min(NB2 - 1, NSG)):
                b2_load(sg)
            for sg in range(NSG):
                i = sg % NB2
                if sg + NB2 - 1 < NSG:
                    b2_load(sg + NB2 - 1)
                av = acc[i][:]
                ak = "acc%d" % i
                S.op("dve", lambda E, av=av, i=i, sg=sg: E.scalar_tensor_tensor(out=av, in0=Y1[i][:], scalar=wts[:, sg, 0:1], in1=av,
                                                                                op0=ALU.mult, op1=ALU.add),
                     reads=["Y1_%d" % i, "wts", ak], writes=[ak])
                S.op("dve", lambda E, av=av, i=i, sg=sg: E.scalar_tensor_tensor(out=av, in0=Y2[i][:], scalar=wts[:, sg, 1:2], in1=av,
                                                                                op0=ALU.mult, op1=ALU.add),
                     reads=["Y2_%d" % i, "wts", ak], writes=[ak])
                S.op("act", lambda E, av=av: E.activation(out=stmp[:], in_=av[:, 0:1024], func=AF.Square, accum_out=small[:, 40:41]),
                     reads=[ak], writes=["stmp", "fss"])
                S.op("act", lambda E, av=av: E.activation(out=stmp[:], in_=av[:, 1024:2048], func=AF.Square, accum_out=small[:, 41:42]),
                     reads=[ak], writes=["stmp", "fss"])
                S.op("dve", lambda E: E.tensor_tensor(out=small[:, 42:43], in0=small[:, 40:41], in1=small[:, 41:42], op=ALU.add),
                     reads=["fss"], writes=["fss2"])
                pow_rstd(small[:, 43:44], small[:, 42:43], 1, 1.0 / D, ["fss2"], ["frstd"])
                S.op("dve", lambda E, av=av: E.scalar_tensor_tensor(out=av, in0=av, scalar=small[:, 43:44], in1=gfin[:],
                                                                   op0=ALU.mult, op1=ALU.mult),
                     reads=[ak, "frstd", "gfin"], writes=[ak])
                S.dma("sp", y[sg * 128:(sg + 1) * 128, :], av, reads=[ak], writes=["y"])

        S.finish()
        with nc.Block() as block:
            @block.tensor
            def _(E):
                S.emit("pe", E)

            @block.scalar
            def _(E):
                S.emit("act", E)

            @block.vector
            def _(E):
                S.emit("dve", E)

            @block.gpsimd
            def _(E):
                S.emit("pool", E)

            @block.sync
            def _(E):
                S.emit("sp", E)
    return nc


def _consts():
    ident = np.eye(128, dtype=np.float32)
    j = np.arange(128)[:, None]
    i = np.arange(128)[None, :]
    same = (j // 64) == (i // 64)
    low = same & (j <= i)
    tri = np.where(low, -1.0 / 16.0, 0.0).astype(np.float32)
    maskT = low.astype(np.float32)
    mask4 = np.tile(maskT, (1, 4)).astype(np.float32)
    q = np.arange(128)[:, None]
    k = np.arange(256)[None, :]
    rel = q + 128 - k
    valid = (rel >= 0) & (rel < 128)
    relm = np.where(valid, -rel.astype(np.float32), -1e9).astype(np.float32)
    relm0 = np.where(valid & (k >= 128), -rel.astype(np.float32), -1e9).astype(np.float32)
    rconst = np.zeros((128, 192), np.float32)
    e = np.arange(NE, dtype=np.float64)[None, :]
    p = np.arange(128, dtype=np.float64)[:, None]
    rconst[:, 0:NE] = e - NE
    triu = (np.arange(128)[:, None] <= np.arange(128)[None, :]).astype(np.float32)
    tokid = (np.arange(16, dtype=np.int32)[None, :] * 128 + np.arange(128, dtype=np.int32)[:, None]).astype(np.int32)
    tabinit = np.full((NSLOT, 1), int(BIG), np.int32)
    return ident, tri, mask4, np.concatenate([relm, relm0], axis=1).astype(np.float32), rconst, triu, tokid, tabinit


_NC_CACHE = {}
GROUP_C0_H = [c * 256 for c in range(12)] + [3088, 3344, 3600, 3856, 4112, 4368]


def kernel(x, norm_mix_g, w_in, w_gk_up, b_gk, gla_norm_g, swa_sinks, w_out, norm_ffn_g, w_group, b_group,
           w_router, b_router, w_gate, w_up, w_down, norm_final_g):
    f = lambda a: np.ascontiguousarray(np.asarray(a), dtype=np.float32)
    x = f(x)
    ident, tri, mask4, relm, rconst, triu, tokid, tabinit = _consts()
    bc = lambda v: np.ascontiguousarray(np.broadcast_to(f(v).reshape(1, -1), (128, f(v).size)))
    common = {
        "w_in_g": np.ascontiguousarray(np.stack([f(w_in)[0][:, c:c + 256] for c in GROUP_C0_H])),
        "w_glr": np.ascontiguousarray(f(w_in)[0][:, 3072:3088]),
        "w_out_g": np.ascontiguousarray(np.stack([f(w_out)[0][:, c * 256:(c + 1) * 256] for c in range(8)])),
        "w_gate": f(w_gate)[0], "w_up": f(w_up)[0], "w_down": f(w_down)[0],
        "w_rt": np.ascontiguousarray(np.concatenate([f(w_group)[0], f(w_router)[0]], axis=1)),
        "g_mix": f(norm_mix_g)[0], "g_ffn": f(norm_ffn_g)[0],
        "gfin_bc": bc(norm_final_g), "gnorm_bc": bc(f(gla_norm_g)[0]), "sinks_bc": bc(f(swa_sinks)[0]),
        "bcat_bc": bc(np.concatenate([f(b_group)[0], f(b_router)[0]])),
        "wgk_aug": np.ascontiguousarray(np.concatenate([f(w_gk_up)[0], f(b_gk)[0][None, :]], axis=0)),
        "ident": ident, "tri": tri, "mask4": mask4, "relm": relm, "rconst": rconst, "triu": triu, "tokid": tokid, "tabinit": tabinit,
    }
    if "nc" not in _NC_CACHE:
        _NC_CACHE["nc"] = build_nc()
    nc = _NC_CACHE["nc"]
    in_maps = [dict(common, x=np.ascontiguousarray(x[b])) for b in range(8)]
    res = run_bass_kernel_spmd(nc, in_maps, core_ids=list(range(8)))
    return np.stack([np.asarray(r["y"], dtype=np.float32) for r in res.results], axis=0)
```

```python
import numpy as np
import ml_dtypes
import concourse.bass as bass
import concourse.mybir as mybir
from concourse.bass_utils import run_bass_kernel_spmd

F32 = mybir.dt.float32
F32R = mybir.dt.float32r
BF16 = mybir.dt.bfloat16
AF = mybir.ActivationFunctionType
ALU = mybir.AluOpType
AX = mybir.AxisListType

D = 2048
T = 2048
TT = 256
NTILE = T // TT
NE = 64
DFF = 256
EPS = 1e-6
INW = 4624
SLOPES = [2.0 ** (-8.0 * (h + 1) / 16.0) for h in range(16)]
NDS = 10
NT_RUN = NTILE
NE_RUN = NE
HALVES = 2
CAP = 128
NSLOT = NE * CAP
BIG = 1.0e6
STOP = None
SKIP_GLA = False
G_ORDER = (0, 1, 2, 3)


class _Rec:
    def __getattr__(self, name):
        return lambda *a, **k: (name, a, k)


_REC = _Rec()


class Sch:
    ENG = ("pe", "act", "dve", "pool", "sp")

    def __init__(self, nc, sem):
        self.nc = nc
        self.sem = sem
        self.prog = {k: [] for k in self.ENG}
        self.cnt = {k: 0 for k in self.ENG}
        self.seen = {k: {} for k in self.ENG}
        self.lastw = {}
        self.readers = {}
        self.dpool = {"sp": ["dsp%d" % i for i in range(NDS)], "pool": ["dpl%d" % i for i in range(NDS)]}
        self.dval = {n: 0 for q in self.dpool for n in self.dpool[q]}
        self.dnext = {"sp": 0, "pool": 0}

    def _deps(self, eng, reads, writes):
        toks = []
        for r in reads:
            t = self.lastw.get(r)
            if t is not None:
                toks.append(t)
        for w in writes:
            t = self.lastw.get(w)
            if t is not None:
                toks.append(t)
            toks.extend(self.readers.get(w, {}).items())
        waits = []
        for (s, v) in toks:
            if s == eng and eng == "pe":
                continue
            if self.seen[eng].get(s, 0) >= v:
                continue
            self.seen[eng][s] = v
            waits.append((s, v))
        return waits

    def _update(self, tok, reads, writes):
        for w in writes:
            self.lastw[w] = tok
            self.readers[w] = {}
        for r in reads:
            d = self.readers.setdefault(r, {})
            if d.get(tok[0], 0) < tok[1]:
                d[tok[0]] = tok[1]

    def mark(self, label):
        if STOP is not None and label == STOP:
            self.dead = True

    def op(self, eng, fn, reads=(), writes=(), signal=True):
        if getattr(self, "dead", False):
            return
        waits = self._deps(eng, reads, writes)
        if signal:
            self.cnt[eng] += 1
            tok = (eng, self.cnt[eng])
        else:
            tok = (eng, self.cnt[eng] + 1)
        self.prog[eng].append((waits, fn(_REC), signal, None))
        self._update(tok, reads, writes)

    def barrier(self):
        for e in self.ENG:
            waits = []
            for o in ("pe", "act", "dve"):
                if o != e and self.cnt[o] > self.seen[e].get(o, 0):
                    waits.append((o, self.cnt[o]))
                    self.seen[e][o] = self.cnt[o]
            for nm, v in self.dval.items():
                if v > self.seen[e].get(nm, 0):
                    waits.append((nm, v))
                    self.seen[e][nm] = v
            self.prog[e].append((waits, None, False, None))

    def dma(self, q, out, in_, reads=(), writes=(), out_off=None, in_off=None, bound="slot"):
        if getattr(self, "dead", False):
            return
        waits = self._deps(q, reads, writes)
        names = self.dpool[q]
        nm = names[self.dnext[q]]
        self.dnext[q] = (self.dnext[q] + 1) % len(names)
        prev = self.dval[nm]
        if prev > 0 and self.seen[q].get(nm, 0) < prev:
            waits.append((nm, prev))
            self.seen[q][nm] = prev
        self.dval[nm] = prev + 16
        tok = (nm, prev + 16)
        if out_off is None and in_off is None:
            self.prog[q].append((waits, ("dma_start", (), dict(out=out, in_=in_)), False, nm))
        else:
            oo = None if out_off is None else bass.IndirectOffsetOnAxis(ap=out_off, axis=0)
            io = None if in_off is None else bass.IndirectOffsetOnAxis(ap=in_off, axis=0)
            self.prog[q].append((waits, ("indirect_dma_start", (), dict(out=out, out_offset=oo, in_=in_, in_offset=io,
                                                                          bounds_check=self.bc_reg[bound], oob_is_err=False)), False, nm))
        self._update(tok, reads, writes)

    def finish(self):
        waits = []
        for nm, v in self.dval.items():
            if v > 0 and self.seen["sp"].get(nm, 0) < v:
                waits.append((nm, v))
        for e in ("pe", "act", "dve"):
            if self.cnt[e] > 0:
                waits.append((e, self.cnt[e]))
        self.prog["sp"].append((waits, None, False, None))

    def emit(self, eng, E):
        for waits, fn, signal, dnm in self.prog[eng]:
            for s, v in waits:
                E.wait_ge(self.sem[s], v)
            if fn is None:
                continue
            ins = getattr(E, fn[0])(*fn[1], **fn[2])
            if signal:
                ins.then_inc(self.sem[eng], 1)
            if dnm is not None:
                ins.then_inc(self.sem[dnm], 16)


def build_nc():
    nc = bass.Bass("TRN2", target_bir_lowering=False)
    dt_ = nc.dram_tensor
    x = dt_("x", [T, D], F32, kind="ExternalInput").ap()
    w_in_g = dt_("w_in_g", [18, D, 256], F32R, kind="ExternalInput").ap()
    w_glr = dt_("w_glr", [D, 16], F32R, kind="ExternalInput").ap()
    w_out_g = dt_("w_out_g", [8, D, 256], F32R, kind="ExternalInput").ap()
    w_gate = dt_("w_gate", [NE, D, DFF], F32R, kind="ExternalInput").ap()
    w_up = dt_("w_up", [NE, D, DFF], F32R, kind="ExternalInput").ap()
    w_down = dt_("w_down", [NE, DFF, D], F32R, kind="ExternalInput").ap()
    w_rt = dt_("w_rt", [D, 68], F32, kind="ExternalInput").ap()
    g_mix = dt_("g_mix", [D], F32, kind="ExternalInput").ap()
    g_ffn = dt_("g_ffn", [D], F32, kind="ExternalInput").ap()
    gfin_d = dt_("gfin_bc", [128, D], F32, kind="ExternalInput").ap()
    gnorm_d = dt_("gnorm_bc", [128, 1024], F32, kind="ExternalInput").ap()
    sinks_d = dt_("sinks_bc", [128, 16], F32, kind="ExternalInput").ap()
    bcat_d = dt_("bcat_bc", [128, 68], F32, kind="ExternalInput").ap()
    wgk_d = dt_("wgk_aug", [17, 512], F32, kind="ExternalInput").ap()
    ident_d = dt_("ident", [128, 128], F32, kind="ExternalInput").ap()
    tri_d = dt_("tri", [128, 128], F32, kind="ExternalInput").ap()
    mask4_d = dt_("mask4", [128, 512], F32, kind="ExternalInput").ap()
    relm_d = dt_("relm", [128, 512], F32, kind="ExternalInput").ap()
    rconst_d = dt_("rconst", [128, 192], F32, kind="ExternalInput").ap()
    triu_d = dt_("triu", [128, 128], F32, kind="ExternalInput").ap()
    h1_d = dt_("h1_scr", [T, D], F32, kind="Internal").ap()
    tokid_d = dt_("tokid", [128, 16], mybir.dt.int32, kind="ExternalInput").ap()
    tabinit_d = dt_("tabinit", [NSLOT, 1], mybir.dt.int32, kind="ExternalInput").ap()
    tab_d = dt_("tab_scr", [NSLOT, 1], mybir.dt.int32, kind="Internal").ap()
    Ys_d = dt_("ys_scr", [NSLOT, D], F32, kind="Internal").ap()
    y = dt_("y", [T, D], F32, kind="ExternalOutput").ap()

    GROUP_C0 = [c * 256 for c in range(12)] + [3088, 3344, 3600, 3856, 4112, 4368]
    w_in_grp = {c0: w_in_g[gi].rearrange("(p k) c -> p k c", k=16) for gi, c0 in enumerate(GROUP_C0)}
    w_out_grp = [w_out_g[gi].rearrange("(p k) c -> p k c", k=16) for gi in range(8)]

    import contextlib
    es = contextlib.ExitStack()
    with es:
        sem = {}
        for nm in list(Sch.ENG) + ["dsp%d" % i for i in range(NDS)] + ["dpl%d" % i for i in range(NDS)]:
            sem[nm] = es.enter_context(nc.semaphore(nm))
        S = Sch(nc, sem)
        S.bc_reg = {"slot": nc.alloc_register(mybir.EngineType.Pool, "bc_slot"), "tok": nc.alloc_register(mybir.EngineType.Pool, "bc_tok")}
        S.prog["pool"].append(([], ("reg_mov", (S.bc_reg["slot"], NSLOT - 1), {}), False, None))
        S.prog["pool"].append(([], ("reg_mov", (S.bc_reg["tok"], T - 1), {}), False, None))

        def sb(name, shape, dtype):
            return es.enter_context(nc.sbuf_tensor("s_" + name, shape, dtype))

        PS = [es.enter_context(nc.psum_tensor("ps%d" % i, [128, 512], F32)) for i in range(8)]

        ridx = sb("ridx", [128, 16, 2], mybir.dt.int32)
        wts = sb("wts", [128, 16, 2], F32)
        tokE = sb("tokE", [128, NE], mybir.dt.int32)
        tokid = sb("tokid", [128, 16], mybir.dt.int32)
        Atot = sb("Atot", [128, NE], F32)
        rconst = sb("rconst", [128, 192], F32)
        triu = sb("triu", [128, 128], F32)
        ones = sb("ones", [128, 128], F32)
        gffnc = sb("gffnc", [128, 16], F32)
        ident = sb("ident", [128, 128], F32)
        identb = sb("identb", [128, 128], BF16)
        small = sb("small", [128, 128], F32)
        SS, RSTD, OSS, ORSTD, MX, NEGM, RSUM, SKA, ESK, RDEN = (slice(0, 2), slice(2, 4), slice(4, 8), slice(8, 12),
                                                                 slice(12, 16), slice(16, 20), slice(20, 24),
                                                                 slice(24, 28), slice(28, 32), slice(32, 36))
        rt = sb("rt", [128, 640], F32)

        S.dma("pool", ident[:], ident_d, writes=["ident"])
        S.dma("pool", rconst[:], rconst_d, writes=["rconst"])
        S.dma("pool", triu[:], triu_d, writes=["triu"])
        S.dma("pool", tokid[:], tokid_d, writes=["tokid"])
        S.dma("pool", tab_d.rearrange("(a b) o -> a (b o)", a=128), tabinit_d.rearrange("(a b) o -> a (b o)", a=128), writes=["tab"])
        S.op("dve", lambda E: E.memset(ones[:], 1.0), writes=["ones"])
        S.op("dve", lambda E: E.memset(Atot[:], 0.0), writes=["Atot"])
        S.op("dve", lambda E: E.tensor_copy(out=identb[:], in_=ident[:]), reads=["ident"], writes=["identb"])

        def pow_rstd(dst, src, n, inv, rk, wk):
            S.op("act", lambda E: E.activation(out=dst, in_=src, func=AF.Ln, scale=inv, bias=EPS), reads=rk, writes=wk)
            S.op("act", lambda E: E.activation(out=dst, in_=dst, func=AF.Exp, scale=-0.5), reads=wk, writes=wk)

        with contextlib.ExitStack() as ea:
            def sa(name, shape, dtype):
                return ea.enter_context(nc.sbuf_tensor("a_" + name, shape, dtype))
            xt = [sa("xt%d" % i, [128, 2, D], F32) for i in range(2)]
            xT = sa("xT", [128, 16, TT], F32R)
            W = [sa("W%d" % i, [128, 16, 256], F32R) for i in range(3)]
            tok = sa("tok", [128, D], F32)
            qT = sa("qT", [128, 4, TT], F32)
            kT = sa("kT", [128, 4, TT], F32)
            vtok = sa("vtok", [128, 2, 1024], BF16)
            rs = sa("rs", [128, 2, 1024], BF16)
            sqT = sa("sqT", [128, 8, TT], BF16)
            skT = sa("skT", [128, 4, 128 + TT], BF16)
            svt = sa("svt", [128, 3, 256], BF16)
            ktok = sa("ktok", [128, 2, 256], BF16)
            ktoks = sa("ktoks", [128, 2, 256], BF16)
            glrT = sa("glrT", [32, TT], F32)
            wglr = sa("wglr", [128, 16, 16], F32R)
            wgk = sa("wgk", [32, 512], F32)
            wr = sa("wr", [128, 16, 68], F32)
            gmixc = sa("gmixc", [128, 16], F32)
            gnorm = sa("gnorm", [128, 1024], F32)
            sinks = sa("sinks", [128, 16], F32)
            bcat = sa("bcat", [128, 68], F32)
            tri = sa("tri", [128, 128], F32)
            mask4 = sa("mask4", [128, 512], F32)
            relm = sa("relm", [128, 512], F32)
            la = sa("la", [128, 512], F32)
            la2 = sa("la2", [128, 512], F32)
            bcs = sa("bcs", [128, 512], F32)
            d1 = sa("d1", [128, 512], F32)
            d4 = sa("d4", [128, 512], F32)
            E1 = sa("E1", [128, 512], F32)
            E2 = sa("E2", [128, 512], F32)
            E3 = sa("E3", [128, 512], F32)
            E4 = sa("E4", [128, 512], F32)
            dec = sa("dec", [128, 8], F32)
            qin = sa("qin", [128, 4, 128], BF16)
            qdec0 = sa("qdec0", [128, 4, 128], BF16)
            qdec1 = sa("qdec1", [128, 4, 128], BF16)
            kin = sa("kin", [128, 4, 128], BF16)
            kdT = sa("kdT", [128, 4, 128], BF16)
            kdtok = sa("kdtok", [128, 4, 128], BF16)
            ATs = sa("ATs", [128, 4, 128], BF16)
            Sst = sa("Sst", [128, 1024], F32)
            Sb = [sa("Sb%d" % i, [128, 1024], BF16) for i in range(2)]
            on = sa("on", [128, 1024], F32)
            junk = on[:].bitcast(BF16)
            ssbs = [sa("ssb%d" % i, [128, 4, 256], F32) for i in range(2)]
            pbs = [sa("pb%d" % i, [128, 4, 256], BF16) for i in range(2)]
            pTs = sa("pTs", [128, 8, 128], BF16)
            lg = sa("lg", [128, 68], F32)

            S.dma("sp", xt[0][:], x[0:TT, :].rearrange("(s p) d -> p s d", p=128), writes=["xt0"])
            S.dma("sp", gmixc[:], g_mix.rearrange("(p k) -> p k", k=16), writes=["gmixc"])
            S.dma("sp", tri[:], tri_d, writes=["tri"])
            S.dma("sp", mask4[:], mask4_d, writes=["mask4"])
            S.dma("sp", relm[:], relm_d, writes=["relm"])
            S.dma("sp", gnorm[:], gnorm_d, writes=["gnorm"])
            S.dma("sp", sinks[:], sinks_d, writes=["sinks"])
            S.dma("sp", bcat[:], bcat_d, writes=["bcat"])
            S.dma("sp", wgk[0:17, :], wgk_d, writes=["wgk"])
            S.dma("sp", wr[:], w_rt.rearrange("(p k) c -> p k c", k=16), writes=["wr"])
            S.dma("sp", gffnc[:], g_ffn.rearrange("(p k) -> p k", k=16), writes=["gffnc"])
            S.dma("pool", wglr[:], w_glr.rearrange("(p k) c -> p k c", k=16), writes=["wglr"])
            S.op("dve", lambda E: E.memset(glrT[:], 1.0), writes=["glrT"])
            S.op("dve", lambda E: E.memset(Sst[:], 0.0), writes=["Sst"])
            S.op("dve", lambda E: E.memset(qdec0[:], 0.0), writes=["qdec"])
            S.op("dve", lambda E: E.memset(qdec1[:], 0.0), writes=["qdec"])
            S.op("dve", lambda E: E.memset(Sb[0][:], 0.0), writes=["Sb0"])
            S.op("dve", lambda E: E.memset(skT[:], 0.0), writes=["skT"])
            S.op("dve", lambda E: E.memset(svt[:], 0.0), writes=["svt"])

            wstate = {"n": 0}

            def wload(src):
                i = wstate["n"] % 3
                wstate["n"] += 1
                S.dma("pool", W[i][:], src, writes=["W%d" % i])
                return i

            mmstate = {"n": 0}

            def mmslot():
                i = mmstate["n"] % 2
                mmstate["n"] += 1
                return PS[i][:, 0:256], "p%d" % i

            trstate = {"n": 0}

            def trslot():
                i = trstate["n"] % 2
                trstate["n"] += 1
                return PS[2 + i], "p%d" % (2 + i)

            evstate = {"n": 0}

            def ev_eng():
                evstate["n"] += 1
                return "act" if evstate["n"] % 2 else "dve"

            def norm_T(src_tile, src_key, gcol, gkey, dst_key):
                for sub in range(2):
                    src = src_tile[:, sub, :]
                    S.op("act", lambda E, src=src, sub=sub: E.activation(out=junk, in_=src, func=AF.Square,
                                                                         accum_out=small[:, sub:sub + 1]),
                         reads=[src_key], writes=["on", "ss"])
                pow_rstd(small[:, RSTD], small[:, SS], 2, 1.0 / D, ["ss"], ["rstd"])
                for sub in range(2):
                    src = src_tile[:, sub, :]
                    S.op("act", lambda E, src=src, sub=sub: E.activation(out=tok[:], in_=src, func=AF.Copy,
                                                                         scale=small[:, 2 + sub:3 + sub]),
                         reads=[src_key, "rstd"], writes=["tok", "tokg", "toks"])
                    transpose_tok(sub, gcol, gkey, dst_key)

            def transpose_tok(sub, gcol, gkey, dst_key):
                tokv = tok[:].rearrange("p (q k) -> p k q", k=16)
                for g4 in range(4):
                    ps, pk = trslot()
                    for i in range(4):
                        kc = g4 * 4 + i
                        S.op("pe", lambda E, ps=ps, i=i, kc=kc: E.transpose(out=ps[:, i * 128:(i + 1) * 128], in_=tokv[:, kc, :],
                                                                            identity=ident[:]),
                             reads=["tok", "tokg", "toks", "ident"], writes=[pk], signal=(i == 3))
                    dst = xT[:, g4 * 4:g4 * 4 + 4, sub * 128:(sub + 1) * 128]
                    psv = ps[:].rearrange("p (a b) -> p a b", a=4)
                    if gcol is not None:
                        gb = gcol[:, g4 * 4:g4 * 4 + 4].unsqueeze(2).to_broadcast([128, 4, 128])
                        S.op("dve", lambda E, dst=dst, psv=psv, gb=gb: E.tensor_tensor(out=dst, in0=psv, in1=gb, op=ALU.mult),
                             reads=[pk, gkey], writes=[dst_key])
                    else:
                        e = ev_eng()
                        if e == "act":
                            S.op("act", lambda E, dst=dst, psv=psv: E.activation(out=dst, in_=psv, func=AF.Copy),
                                 reads=[pk], writes=[dst_key])
                        else:
                            S.op("dve", lambda E, dst=dst, psv=psv: E.tensor_copy(out=dst, in_=psv), reads=[pk], writes=[dst_key])

            def load_x(t):
                b = t % 2
                S.dma("sp", xt[b][:], x[t * TT:(t + 1) * TT, :].rearrange("(s p) d -> p s d", p=128), writes=["xt%d" % b])

            pending_scatter = []

            def flush_scatter():
                while pending_scatter:
                    sg_, k_ = pending_scatter.pop(0)
                    S.dma("pool", tab_d[:, :], tokid[:, sg_:sg_ + 1], reads=["tokid", "ridx"], writes=["tab"], out_off=ridx[:, sg_, k_:k_ + 1])

            S.mark("const")
            for t in range(NT_RUN):
                b = t % 2
                xb = xt[b]
                xk = "xt%d" % b
                if t + 1 < NT_RUN:
                    load_x(t + 1)
                norm_T(xb, xk, gmixc, "gmixc", "xT")

                S.mark("norm1")
                ps, pk = mmslot()
                for kc in range(16):
                    S.op("pe", lambda E, ps=ps, kc=kc: E.matmul(ps[0:16, :], lhsT=wglr[:, kc, :], rhs=xT[:, kc, :],
                                                               start=(kc == 0), stop=(kc == 15)),
                         reads=["wglr", "xT"], writes=[pk], signal=(kc == 15))
                S.op("act", lambda E, ps=ps: E.activation(out=glrT[0:16, :], in_=ps[0:16, :], func=AF.Copy),
                     reads=[pk], writes=["glrT"])

                def fm_group(c0, evac):
                    wi = wload(w_in_grp[c0])
                    for j in range(2):
                        ps, pk = mmslot()
                        for kc in range(16):
                            S.op("pe", lambda E, ps=ps, kc=kc, wi=wi, j=j: E.matmul(ps, lhsT=W[wi][:, kc, j * 128:(j + 1) * 128],
                                                                                 rhs=xT[:, kc, :], start=(kc == 0), stop=(kc == 15)),
                                 reads=["W%d" % wi, "xT"], writes=[pk], signal=(kc == 15))
                        evac(j, ps, pk)

                def tm_group(c0, evac):
                    wi = wload(w_in_grp[c0])
                    for sub in range(2):
                        ps, pk = mmslot()
                        for kc in range(16):
                            S.op("pe", lambda E, ps=ps, kc=kc, wi=wi, sub=sub: E.matmul(ps, lhsT=xT[:, kc, sub * 128:(sub + 1) * 128],
                                                                                     rhs=W[wi][:, kc, :], start=(kc == 0), stop=(kc == 15)),
                                 reads=["W%d" % wi, "xT"], writes=[pk], signal=(kc == 15))
                        evac(sub, ps, pk)

                for gi in range(2):
                    def ev(j, ps, pk, gi=gi):
                        h = gi * 2 + j
                        S.op("act", lambda E: E.activation(out=qT[:, h, :], in_=ps, func=AF.Copy, scale=128.0 ** -0.5),
                             reads=[pk], writes=["qT"])
                    fm_group(gi * 256, ev)
                for gi in range(2):
                    def ev(j, ps, pk, gi=gi):
                        h = gi * 2 + j
                        S.op("dve", lambda E: E.tensor_copy(out=kT[:, h, :], in_=ps), reads=[pk], writes=["kT"])
                    fm_group(512 + gi * 256, ev)
                flush_scatter()
                for gi in range(4):
                    def ev(sub, ps, pk, gi=gi):
                        S.op("dve", lambda E: E.tensor_copy(out=vtok[:, sub, gi * 256:(gi + 1) * 256], in_=ps),
                             reads=[pk], writes=["vtok"])
                    tm_group(1024 + gi * 256, ev)
                for gi in range(4):
                    def ev(sub, ps, pk, gi=gi):
                        S.op("act", lambda E: E.activation(out=rs[:, sub, gi * 256:(gi + 1) * 256], in_=ps, func=AF.Silu),
                             reads=[pk], writes=["rs"])
                    tm_group(2048 + gi * 256, ev)
                for gi in range(4):
                    def ev(j, ps, pk, gi=gi):
                        c = gi * 2 + j
                        S.op("dve", lambda E: E.tensor_scalar(out=sqT[:, c, :], in0=ps, scalar1=0.125, scalar2=None, op0=ALU.mult),
                             reads=[pk], writes=["sqT"])
                    fm_group(3088 + gi * 256, ev)
                def ev(sub, ps, pk):
                    S.op("act", lambda E: E.activation(out=ktok[:, sub, :], in_=ps, func=AF.Copy), reads=[pk], writes=["ktok"])
                    kv4 = ktok[:, sub, :].rearrange("p (a b c) -> p a b c", a=2, b=2)
                    ks4 = ktoks[:, sub, :].rearrange("p (a b c) -> p a b c", a=2, b=2)
                    S.op("dve", lambda E: E.tensor_copy(out=ks4[:, :, 0, :], in_=kv4[:, :, 1, :]), reads=["ktok"], writes=["ktoks"])
                    S.op("dve", lambda E: E.tensor_copy(out=ks4[:, :, 1, :], in_=kv4[:, :, 0, :]), reads=["ktok"], writes=["ktoks"])
                    trp, trk = trslot()
                    trb = trp[:].bitcast(BF16)
                    srcs = [ktok[:, sub, 0:128], ktoks[:, sub, 0:128], ktok[:, sub, 128:256], ktoks[:, sub, 128:256]]
                    for i in range(4):
                        S.op("pe", lambda E, i=i: E.transpose(out=trb[:, i * 128:(i + 1) * 128], in_=srcs[i], identity=identb[:]),
                             reads=["ktok", "ktoks", "identb"], writes=[trk], signal=(i == 3))
                    c0 = 128 + sub * 128
                    tv = trb[:, 0:512].rearrange("p (g q) -> p g q", g=4)
                    S.op("act", lambda E: E.activation(out=skT[0:64, :, c0:c0 + 128], in_=tv[0:64, :, :], func=AF.Copy),
                         reads=[trk], writes=["skT"])
                    tv2 = trb[:, 0:512].rearrange("p (a b q) -> p a b q", a=2, b=2)
                    sk2 = skT[:, :, c0:c0 + 128].rearrange("p (a b) q -> p a b q", a=2)
                    S.op("dve", lambda E: E.tensor_copy(out=sk2[64:128, :, 0, :], in_=tv2[64:128, :, 1, :]), reads=[trk], writes=["skT"])
                    S.op("dve", lambda E: E.tensor_copy(out=sk2[64:128, :, 1, :], in_=tv2[64:128, :, 0, :]), reads=[trk], writes=["skT"])
                tm_group(4112, ev)

                def ev(sub, ps, pk):
                    S.op("dve", lambda E: E.tensor_copy(out=svt[:, 1 + sub, :], in_=ps), reads=[pk], writes=["svt"])
                tm_group(4368, ev)

                S.mark("inproj")
                wo_idx = []
                for dg in range(2):
                    wo_idx.append(wload(w_out_grp[dg]))

                def gla_gen(sub):
                    ts = slice(sub * 128, (sub + 1) * 128)
                    zp = PS[7]
                    S.op("pe", lambda E: E.matmul(zp[:], lhsT=glrT[0:17, ts], rhs=wgk[0:17, :], start=True, stop=True),
                         reads=["glrT", "wgk"], writes=["p7"])
                    S.op("act", lambda E: E.activation(out=la2[:], in_=zp[:], func=AF.Exp, scale=-1.0), reads=["p7"], writes=["la2"])
                    S.op("act", lambda E: E.activation(out=la[:], in_=la2[:], func=AF.Ln, bias=1.0), reads=["la2"], writes=["la"])
                    yield
                    bp = PS[7]
                    for h in range(4):
                        S.op("pe", lambda E, h=h: E.matmul(bp[:, h * 128:(h + 1) * 128], lhsT=la[:, h * 128:(h + 1) * 128], rhs=tri[:],
                                                           start=True, stop=True),
                             reads=["la", "tri"], writes=["p7"], signal=(h == 3))
                    S.op("act", lambda E: E.activation(out=bcs[:], in_=bp[:], func=AF.Copy), reads=["p7"], writes=["bcs"])
                    yield
                    bv = bcs[:].rearrange("p (a b) -> p a b", b=64)
                    d1v = d1[:].rearrange("p (a b) -> p a b", b=64)
                    d4v = d4[:].rearrange("p (a b) -> p a b", b=64)
                    S.op("dve", lambda E: E.tensor_tensor(out=d1v, in0=bv, in1=bv[:, :, 31:32].to_broadcast([128, 8, 64]), op=ALU.subtract),
                         reads=["bcs"], writes=["d1"])
                    S.op("dve", lambda E: E.tensor_tensor(out=d4v, in0=bv, in1=bv[:, :, 63:64].to_broadcast([128, 8, 64]), op=ALU.subtract),
                         reads=["bcs"], writes=["d4"])
                    S.op("act", lambda E: E.activation(out=E1[:], in_=d1[:], func=AF.Exp), reads=["d1"], writes=["E1"])
                    S.op("act", lambda E: E.activation(out=E2[:], in_=d1[:], func=AF.Exp, scale=-1.0), reads=["d1"], writes=["E2"])
                    S.op("act", lambda E: E.activation(out=E3[:], in_=bcs[:], func=AF.Exp), reads=["bcs"], writes=["E3"])
                    S.op("act", lambda E: E.activation(out=E4[:], in_=d4[:], func=AF.Exp, scale=-1.0), reads=["d4"], writes=["E4"])
                    S.op("act", lambda E: E.activation(out=dec[:].unsqueeze(2), in_=bv[:, :, 63:64], func=AF.Exp), reads=["bcs"], writes=["dec"])
                    yield
                    qv = qT[:, :, ts]
                    kv_ = kT[:, :, ts]
                    e4 = lambda a: a[:].rearrange("p (h i) -> p h i", h=4)
                    S.op("dve", lambda E: E.tensor_tensor(out=qin[:], in0=qv, in1=e4(E1), op=ALU.mult), reads=["qT", "E1"], writes=["qin"])
                    S.op("dve", lambda E: E.tensor_tensor(out=kin[:], in0=kv_, in1=e4(E2), op=ALU.mult), reads=["kT", "E2"], writes=["kin"])
                    S.op("dve", lambda E: E.tensor_tensor(out=qdec0[:, :, 0:64], in0=qv[:, :, 0:64], in1=e4(E3)[:, :, 0:64], op=ALU.mult),
                         reads=["qT", "E3"], writes=["qdec"])
                    S.op("dve", lambda E: E.tensor_tensor(out=qdec1[:, :, 64:128], in0=qv[:, :, 64:128], in1=e4(E3)[:, :, 64:128], op=ALU.mult),
                         reads=["qT", "E3"], writes=["qdec"])
                    S.op("dve", lambda E: E.tensor_tensor(out=kdT[:], in0=kv_, in1=e4(E4), op=ALU.mult), reads=["kT", "E4"], writes=["kdT"])
                    yield
                    ap_ = PS[7]
                    for h in range(4):
                        S.op("pe", lambda E, h=h: E.matmul(ap_[:, h * 128:(h + 1) * 128], lhsT=kin[:, h, :], rhs=qin[:, h, :], start=True, stop=True),
                             reads=["kin", "qin"], writes=["p7"], signal=(h == 3))
                    trp, trk = trslot()
                    trb = trp[:].bitcast(BF16)
                    for h in range(4):
                        S.op("pe", lambda E, h=h, trb=trb: E.transpose(out=trb[:, h * 128:(h + 1) * 128], in_=kdT[:, h, :], identity=identb[:]),
                             reads=["kdT", "identb"], writes=[trk], signal=(h == 3))
                    S.op("dve", lambda E: E.tensor_tensor(out=ATs[:].rearrange("p h i -> p (h i)"), in0=ap_[:], in1=mask4[:], op=ALU.mult),
                         reads=["p7", "mask4"], writes=["ATs"])
                    S.op("act", lambda E, trb=trb: E.activation(out=kdtok[:].rearrange("p h i -> p (h i)"), in_=trb[:, 0:512], func=AF.Copy),
                         reads=[trk], writes=["kdtok"])
                    yield

                    def kv_update(c):
                        cs = slice(c * 64, (c + 1) * 64)
                        for hp in range(2):
                            for hh in range(2):
                                h = hp * 2 + hh
                                kp = PS[6][:, hh * 256:hh * 256 + 256]
                                S.op("pe", lambda E, h=h, kp=kp: E.matmul(kp, lhsT=kdtok[cs, h, :], rhs=vtok[cs, sub, h * 256:(h + 1) * 256],
                                                                          start=True, stop=True),
                                     reads=["kdtok", "vtok"], writes=["p6"], signal=(hh == 1))
                            for hh in range(2):
                                h = hp * 2 + hh
                                kp = PS[6][:, hh * 256:hh * 256 + 256]
                                sv_ = Sst[:, h * 256:(h + 1) * 256]
                                S.op("dve", lambda E, h=h, kp=kp, sv_=sv_: E.scalar_tensor_tensor(
                                    out=sv_, in0=sv_, scalar=dec[:, h * 2 + c:h * 2 + c + 1], in1=kp, op0=ALU.mult, op1=ALU.add),
                                     reads=["Sst", "dec", "p6"], writes=["Sst"])
                        tgt = Sb[(c + 1) % 2]
                        S.op("act", lambda E: E.activation(out=tgt[:], in_=Sst[:], func=AF.Copy),
                             reads=["Sst"], writes=["Sb%d" % ((c + 1) % 2)])

                    kv_update(0)
                    yield
                    for h in range(4):
                        op_ = PS[(h // 2)][:, (h % 2) * 256:(h % 2) * 256 + 256]
                        ok_ = "p%d" % (h // 2)
                        S.op("pe", lambda E, h=h, op_=op_: E.matmul(op_, lhsT=ATs[:, h, :], rhs=vtok[:, sub, h * 256:(h + 1) * 256],
                                                                   start=True, stop=False),
                             reads=["ATs", "vtok"], writes=[ok_], signal=False)
                        S.op("pe", lambda E, h=h, op_=op_: E.matmul(op_, lhsT=qdec0[:, h, :], rhs=Sb[0][:, h * 256:(h + 1) * 256],
                                                                   start=False, stop=False),
                             reads=["qdec", "Sb0"], writes=[ok_], signal=False)
                        S.op("pe", lambda E, h=h, op_=op_: E.matmul(op_, lhsT=qdec1[:, h, :], rhs=Sb[1][:, h * 256:(h + 1) * 256],
                                                                   start=False, stop=True),
                             reads=["qdec", "Sb1"], writes=[ok_], signal=True)
                    yield
                    kv_update(1)
                    yield
                    for h in range(4):
                        op_ = PS[(h // 2)][:, (h % 2) * 256:(h % 2) * 256 + 256]
                        S.op("act", lambda E, h=h, op_=op_: E.activation(out=junk[:, 0:256], in_=op_, func=AF.Square,
                                                                       accum_out=small[:, 4 + h:5 + h]),
                             reads=["p%d" % (h // 2)], writes=["on", "oss"])
                    pow_rstd(small[:, ORSTD], small[:, OSS], 4, 1.0 / 256, ["oss"], ["orstd"])
                    for h in range(4):
                        op_ = PS[(h // 2)][:, (h % 2) * 256:(h % 2) * 256 + 256]
                        S.op("dve", lambda E, h=h, op_=op_: E.scalar_tensor_tensor(
                            out=on[:, h * 256:(h + 1) * 256], in0=op_, scalar=small[:, 8 + h:9 + h], in1=gnorm[:, h * 256:(h + 1) * 256],
                            op0=ALU.mult, op1=ALU.mult),
                             reads=["p%d" % (h // 2), "orstd", "gnorm"], writes=["on"])
                    S.op("dve", lambda E: E.tensor_tensor(out=tok[:, 0:1024], in0=on[:], in1=rs[:, sub, :], op=ALU.mult),
                         reads=["on", "rs"], writes=["tokg"])

                def swa_gen(sub, g):
                    ts = slice(sub * 128, (sub + 1) * 128)
                    nbg = t * 2 + sub
                    rl = relm[:, 256:512] if nbg == 0 else relm[:, 0:256]
                    par = g % 2
                    ssb, pb = ssbs[par], pbs[par]
                    so = 64 * par
                    sl = lambda x_: slice(x_.start + so, x_.stop + so)
                    kx = lambda nm: "%s%d" % (nm, par)
                    for j in range(4):
                        h = 4 * g + j
                        c = h // 2
                        base = (h % 2) * 64
                        sp_ = PS[4 + j % 2][:, (j // 2) * 256:(j // 2) * 256 + 256]
                        S.op("pe", lambda E, c=c, base=base, sp_=sp_: E.matmul(
                            sp_, lhsT=sqT[base:base + 64, c, ts], rhs=skT[base:base + 64, g, sub * 128:sub * 128 + 256],
                            start=True, stop=True),
                             reads=["sqT", "skT"], writes=["p%d" % (4 + j % 2)], signal=(j >= 2))
                    for j in range(4):
                        h = 4 * g + j
                        sp_ = PS[4 + j % 2][:, (j // 2) * 256:(j // 2) * 256 + 256]
                        S.op("dve", lambda E, j=j, h=h, sp_=sp_: E.scalar_tensor_tensor(
                            out=ssb[:, j, :], in0=rl, scalar=SLOPES[h], in1=sp_, op0=ALU.mult, op1=ALU.add),
                             reads=["relm", "p%d" % (4 + j % 2)], writes=[kx("ssb")])
                    yield
                    S.op("dve", lambda E: E.tensor_reduce(out=small[:, sl(MX)], in_=ssb[:], axis=AX.X, op=ALU.max),
                         reads=[kx("ssb")], writes=[kx("mx")])
                    S.op("dve", lambda E: E.tensor_tensor(out=small[:, sl(MX)], in0=small[:, sl(MX)], in1=sinks[:, 4 * g:4 * g + 4], op=ALU.max),
                         reads=[kx("mx"), "sinks"], writes=[kx("mx")])
                    S.op("dve", lambda E: E.tensor_scalar(out=small[:, sl(NEGM)], in0=small[:, sl(MX)], scalar1=-1.0, scalar2=None, op0=ALU.mult),
                         reads=[kx("mx")], writes=[kx("negm")])
                    S.op("dve", lambda E: E.tensor_tensor(out=small[:, sl(SKA)], in0=sinks[:, 4 * g:4 * g + 4], in1=small[:, sl(MX)], op=ALU.subtract),
                         reads=[kx("mx"), "sinks"], writes=[kx("ska")])
                    for j in range(4):
                        S.op("act", lambda E, j=j: E.activation(out=pb[:, j, :], in_=ssb[:, j, :], func=AF.Exp,
                                                                bias=small[:, so + 16 + j:so + 17 + j], scale=1.0,
                                                                accum_out=small[:, so + 20 + j:so + 21 + j]),
                             reads=[kx("ssb"), kx("negm")], writes=[kx("pb"), kx("rsum")])
                    S.op("act", lambda E: E.activation(out=small[:, sl(ESK)], in_=small[:, sl(SKA)], func=AF.Exp), reads=[kx("ska")], writes=[kx("esk")])
                    yield
                    S.op("dve", lambda E: E.tensor_tensor(out=small[:, sl(RDEN)], in0=small[:, sl(RSUM)], in1=small[:, sl(ESK)], op=ALU.add),
                         reads=[kx("rsum"), kx("esk")], writes=[kx("rden")])
                    S.op("dve", lambda E: E.reciprocal(out=small[:, sl(RDEN)], in_=small[:, sl(RDEN)]), reads=[kx("rden")], writes=[kx("rden")])
                    trp, trk = trslot()
                    trb = trp[:].bitcast(BF16)
                    for j in range(4):
                        for kb in range(2):
                            i = j * 2 + kb
                            S.op("pe", lambda E, j=j, kb=kb, i=i, trb=trb: E.transpose(
                                out=trb[:, i * 128:(i + 1) * 128], in_=pb[:, j, kb * 128:(kb + 1) * 128], identity=identb[:]),
                                 reads=[kx("pb"), "identb"], writes=[trk], signal=(i == 7))
                    S.op("act", lambda E, trb=trb: E.activation(out=pTs[:].rearrange("p a b -> p (a b)"), in_=trb, func=AF.Copy),
                         reads=[trk], writes=["pTs"])
                    yield
                    op_ = PS[6][:, 0:256]
                    for j in range(4):
                        for kb in range(2):
                            S.op("pe", lambda E, j=j, kb=kb: E.matmul(
                                op_[:, j * 64:(j + 1) * 64], lhsT=pTs[:, j * 2 + kb, :], rhs=svt[:, sub + kb, g * 64:(g + 1) * 64],
                                start=(kb == 0), stop=(kb == 1)),
                                 reads=["pTs", "svt"], writes=["p6"], signal=(j == 3 and kb == 1))
                    S.op("dve", lambda E: E.tensor_tensor(
                        out=tok[:, 1024 + g * 256:1024 + (g + 1) * 256].rearrange("p (a b) -> p a b", a=4),
                        in0=op_.rearrange("p (a b) -> p a b", a=4),
                        in1=small[:, sl(RDEN)].unsqueeze(2).to_broadcast([128, 4, 64]), op=ALU.mult),
                         reads=["p6", kx("rden")], writes=["toks"])
                    yield

                for sub in range(2):
                    gens = {"gla": gla_gen(sub)}
                    for g in range(4):
                        gens[g] = swa_gen(sub, g)
                    live = set(gens.keys())
                    rnd = 0
                    while live:
                        for key in ["gla", 0, 1, 2, 3]:
                            if key not in live:
                                continue
                            if key != "gla" and rnd < 2 * key:
                                continue
                            try:
                                next(gens[key])
                            except StopIteration:
                                live.discard(key)
                        rnd += 1
                    transpose_tok(sub, None, None, "xT")

                S.mark("mixT")
                S.op("dve", lambda E: E.tensor_copy(out=skT[:, :, 0:128], in_=skT[:, :, TT:TT + 128]), reads=["skT"], writes=["skT"])
                S.op("dve", lambda E: E.tensor_copy(out=svt[:, 0, :], in_=svt[:, 2, :]), reads=["svt"], writes=["svt"])

                for dg in range(8):
                    if dg + 2 < 8:
                        wo_idx.append(wload(w_out_grp[dg + 2]))
                    wi = wo_idx[dg]
                    for sub in range(2):
                        ps, pk = mmslot()
                        for c in range(16):
                            S.op("pe", lambda E, ps=ps, c=c, wi=wi, sub=sub: E.matmul(ps, lhsT=xT[:, c, sub * 128:(sub + 1) * 128],
                                                                                   rhs=W[wi][:, c, :], start=(c == 0), stop=(c == 15)),
                                 reads=["W%d" % wi, "xT"], writes=[pk], signal=(c == 15))
                        dst = xb[:, sub, dg * 256:(dg + 1) * 256]
                        S.op("dve", lambda E, ps=ps, dst=dst: E.tensor_tensor(out=dst, in0=ps, in1=dst, op=ALU.add),
                             reads=[pk, xk], writes=[xk])
                S.dma("sp", h1_d[t * TT:(t + 1) * TT, :].rearrange("(s p) d -> p s d", p=128), xb[:], reads=[xk], writes=["h1d"])

                S.mark("outproj")
                norm_T(xb, xk, gffnc, "gffnc", "xT")
                for sub in range(2):
                    sg = t * 2 + sub
                    ps, pk = mmslot()
                    for kc in range(16):
                        S.op("pe", lambda E, ps=ps, kc=kc, sub=sub: E.matmul(ps[:, 0:68], lhsT=xT[:, kc, sub * 128:(sub + 1) * 128].bitcast(F32),
                                                                          rhs=wr[:, kc, :], start=(kc == 0), stop=(kc == 15)),
                             reads=["xT", "wr"], writes=[pk], signal=(kc == 15))
                    S.op("dve", lambda E, ps=ps: E.tensor_tensor(out=lg[:], in0=ps[:, 0:68], in1=bcat[:], op=ALU.add),
                         reads=[pk, "bcat"], writes=["lg"])
                    R = lambda a, b: rt[:, a:b]
                    GM, GOH, GE, GS, SEL, M1, OH1, SEL2, M2, OH2, DL, W1, W2, WI = (R(0, 1), R(4, 8), R(8, 12), R(12, 13), R(16, 32), R(32, 33),
                                                                                 R(48, 64), R(64, 80), R(80, 81), R(96, 112), R(112, 113),
                                                                                 R(113, 114), R(114, 115), R(128, 144))
                    NGM = R(1, 2)
                    dv = lambda fn, r, w: S.op("dve", fn, reads=r, writes=w)
                    dv(lambda E: E.tensor_reduce(out=GM, in_=lg[:, 0:4], axis=AX.X, op=ALU.max), ["lg"], ["rt"])
                    dv(lambda E: E.tensor_scalar(out=GOH, in0=lg[:, 0:4], scalar1=GM, scalar2=None, op0=ALU.is_equal), ["lg", "rt"], ["rt"])
                    dv(lambda E: E.tensor_scalar(out=NGM, in0=GM, scalar1=-1.0, scalar2=None, op0=ALU.mult), ["rt"], ["rt"])
                    S.op("act", lambda E: E.activation(out=GE, in_=lg[:, 0:4], func=AF.Exp, bias=NGM, scale=1.0, accum_out=GS),
                         reads=["lg", "rt"], writes=["rt"])
                    dv(lambda E: E.reciprocal(out=GS, in_=GS), ["rt"], ["rt"])
                    dv(lambda E: E.tensor_scalar(out=SEL, in0=lg[:, 4:20], scalar1=rt[:, 4:5], scalar2=None, op0=ALU.mult), ["lg", "rt"], ["rt"])
                    for g in range(1, 4):
                        dv(lambda E, g=g: E.scalar_tensor_tensor(out=SEL, in0=lg[:, 4 + 16 * g:20 + 16 * g], scalar=rt[:, 4 + g:5 + g], in1=SEL,
                                                                 op0=ALU.mult, op1=ALU.add), ["lg", "rt"], ["rt"])
                    dv(lambda E: E.tensor_reduce(out=M1, in_=SEL, axis=AX.X, op=ALU.max), ["rt"], ["rt"])
                    dv(lambda E: E.tensor_scalar(out=OH1, in0=SEL, scalar1=M1, scalar2=None, op0=ALU.is_equal), ["rt"], ["rt"])
                    dv(lambda E: E.scalar_tensor_tensor(out=SEL2, in0=OH1, scalar=-1e30, in1=SEL, op0=ALU.mult, op1=ALU.add), ["rt"], ["rt"])
                    dv(lambda E: E.tensor_reduce(out=M2, in_=SEL2, axis=AX.X, op=ALU.max), ["rt"], ["rt"])
                    dv(lambda E: E.tensor_scalar(out=OH2, in0=SEL2, scalar1=M2, scalar2=None, op0=ALU.is_equal), ["rt"], ["rt"])
                    dv(lambda E: E.tensor_tensor(out=DL, in0=M2, in1=M1, op=ALU.subtract), ["rt"], ["rt"])
                    S.op("act", lambda E: E.activation(out=W2, in_=DL, func=AF.Exp), reads=["rt"], writes=["rt"])
                    dv(lambda E: E.tensor_scalar(out=W1, in0=W2, scalar1=1.0, scalar2=None, op0=ALU.add), ["rt"], ["rt"])
                    dv(lambda E: E.reciprocal(out=W1, in_=W1), ["rt"], ["rt"])
                    dv(lambda E: E.tensor_tensor(out=W2, in0=W2, in1=W1, op=ALU.mult), ["rt"], ["rt"])
                    dv(lambda E: E.tensor_tensor(out=W1, in0=W1, in1=GS, op=ALU.mult), ["rt"], ["rt"])
                    dv(lambda E: E.tensor_tensor(out=W2, in0=W2, in1=GS, op=ALU.mult), ["rt"], ["rt"])
                    dv(lambda E: E.tensor_scalar(out=WI, in0=OH1, scalar1=W1, scalar2=None, op0=ALU.mult), ["rt"], ["rt"])
                    dv(lambda E: E.scalar_tensor_tensor(out=WI, in0=OH2, scalar=W2, in1=WI, op0=ALU.mult, op1=ALU.add), ["rt"], ["rt"])
                    A1, A2, AA, TMP, OVF, P1, RF, OKK = (R(192, 256), R(256, 320), R(320, 384), R(384, 448), R(448, 512),
                                                         R(512, 576), R(576, 578), R(578, 580))
                    a3 = lambda a: a.rearrange("p (g j) -> p g j", g=4)
                    gb3 = GOH.unsqueeze(2).to_broadcast([128, 4, 16])
                    dv(lambda E: E.tensor_tensor(out=a3(A1), in0=gb3, in1=OH1.unsqueeze(1).to_broadcast([128, 4, 16]), op=ALU.mult),
                       ["rt"], ["rt"])
                    dv(lambda E: E.tensor_tensor(out=a3(A2), in0=gb3, in1=OH2.unsqueeze(1).to_broadcast([128, 4, 16]), op=ALU.mult),
                       ["rt"], ["rt"])
                    dv(lambda E: E.tensor_tensor(out=AA, in0=A1, in1=A2, op=ALU.add), ["rt"], ["rt"])
                    ps, pk = mmslot()
                    S.op("pe", lambda E, ps=ps: E.matmul(ps[:, 0:NE], lhsT=triu[:], rhs=AA, start=True, stop=False),
                         reads=["rt", "triu"], writes=[pk], signal=False)
                    S.op("pe", lambda E, ps=ps: E.matmul(ps[:, 0:NE], lhsT=ones[:], rhs=Atot[:], start=False, stop=True),
                         reads=["Atot", "ones"], writes=[pk], signal=True)
                    dv(lambda E, ps=ps: E.tensor_scalar(out=OVF, in0=ps[:, 0:NE], scalar1=CAP + 0.5, scalar2=BIG, op0=ALU.is_gt, op1=ALU.mult),
                       [pk], ["rt"])
                    dv(lambda E, ps=ps: E.scalar_tensor_tensor(out=TMP, in0=ps[:, 0:NE], scalar=float(NE), in1=rconst[:, 0:NE],
                                                               op0=ALU.mult, op1=ALU.add), [pk, "rconst"], ["rt"])
                    dv(lambda E: E.tensor_tensor(out=TMP, in0=TMP, in1=OVF, op=ALU.add), ["rt"], ["rt"])
                    dv(lambda E: E.tensor_tensor(out=P1, in0=A1, in1=TMP, op=ALU.mult), ["rt"], ["rt"])
                    dv(lambda E: E.tensor_reduce(out=RF[:, 0:1], in_=P1, axis=AX.X, op=ALU.add), ["rt"], ["rt"])
                    dv(lambda E: E.tensor_tensor(out=P1, in0=A2, in1=TMP, op=ALU.mult), ["rt"], ["rt"])
                    dv(lambda E: E.tensor_reduce(out=RF[:, 1:2], in_=P1, axis=AX.X, op=ALU.add), ["rt"], ["rt"])
                    dv(lambda E, sg=sg: E.tensor_copy(out=ridx[:, sg, :], in_=RF), ["rt"], ["ridx"])
                    dv(lambda E: E.tensor_scalar(out=OKK, in0=RF, scalar1=BIG * 0.5, scalar2=None, op0=ALU.is_lt), ["rt"], ["rt"])
                    dv(lambda E, sg=sg: E.tensor_tensor(out=wts[:, sg, :], in0=rt[:, 113:115], in1=OKK, op=ALU.mult), ["rt"], ["wts"])
                    dv(lambda E: E.tensor_tensor(out=Atot[:], in0=Atot[:], in1=AA, op=ALU.add), ["rt", "Atot"], ["Atot"])
                    for k in range(2):
                        pending_scatter.append((sg, k))

            flush_scatter()

        S.barrier()
        with contextlib.ExitStack() as eb:
            def sm(name, shape, dtype):
                return eb.enter_context(nc.sbuf_tensor("b_" + name, shape, dtype))
            Xg = [sm("Xg%d" % i, [128, D], F32) for i in range(2)]
            XT = [sm("XT%d" % i, [128, 16, 128], F32R) for i in range(2)]
            WG = [sm("WG%d" % i, [128, 16, DFF], F32R) for i in range(2)]
            WU = [sm("WU%d" % i, [128, 16, DFF], F32R) for i in range(2)]
            WD = [sm("WD%d" % i, [128, 2, D], F32R) for i in range(2)]
            Yb = [sm("Yb%d" % i, [128, D], F32) for i in range(2)]
            SG = sm("SG", [128, DFF], F32)
            Hh = sm("Hh", [128, DFF], F32)
            HT = sm("HT", [128, 2, 128], F32R)
            junkb = sm("junkb", [128, D], BF16)
            wg_r = [w_gate[e].rearrange("(p k) f -> p k f", k=16) for e in range(NE)]
            wu_r = [w_up[e].rearrange("(p k) f -> p k f", k=16) for e in range(NE)]
            wd_r = [w_down[e].rearrange("(p c) d -> p c d", c=2) for e in range(NE)]
            for i in range(2):
                S.op("dve", lambda E, i=i: E.memset(Xg[i][:], 0.0), writes=["Xg%d" % i])
            S.dma("pool", tokE[:], tab_d.rearrange("(s e) o -> s (e o)", e=NE), reads=["tab"], writes=["tokE"])
            ys_r = Ys_d.rearrange("(s e) d -> e s d", e=NE)

            def b_load_gu(e):
                i = e % 2
                S.dma("pool", WG[i][:], wg_r[e], writes=["WG%d" % i])
                S.dma("pool", WU[i][:], wu_r[e], writes=["WU%d" % i])
                S.dma("pool", Xg[i][:, :], h1_d[:, :], reads=["h1d", "tokE"], writes=["Xg%d" % i], in_off=tokE[:, e:e + 1], bound="tok")

            def b_load_d(e):
                i = e % 2
                S.dma("pool", WD[i][:], wd_r[e], writes=["WD%d" % i])

            def b_transpose(e):
                i = e % 2
                S.op("act", lambda E: E.activation(out=junkb[:], in_=Xg[i][:], func=AF.Square, accum_out=small[:, 44 + i:45 + i]),
                     reads=["Xg%d" % i], writes=["junkb", "bss%d" % i])
                pow_rstd(small[:, 46 + i:47 + i], small[:, 44 + i:45 + i], 1, 1.0 / D, ["bss%d" % i], ["brstd%d" % i])
                xv = Xg[i][:].rearrange("p (q k) -> p k q", k=16)
                for g4 in range(4):
                    ps, pk = PS[g4 % 2], "p%d" % (g4 % 2)
                    for j in range(4):
                        kc = g4 * 4 + j
                        S.op("pe", lambda E, ps=ps, j=j, kc=kc: E.transpose(out=ps[:, j * 128:(j + 1) * 128], in_=xv[:, kc, :], identity=ident[:]),
                             reads=["Xg%d" % i, "ident"], writes=[pk], signal=(j == 3))
                    dst = XT[i][:, g4 * 4:g4 * 4 + 4, :]
                    psv = ps[:].rearrange("p (a b) -> p a b", a=4)
                    if g4 % 2 == 0:
                        gb = gffnc[:, g4 * 4:g4 * 4 + 4].unsqueeze(2).to_broadcast([128, 4, 128])
                        S.op("dve", lambda E, dst=dst, psv=psv, gb=gb: E.tensor_tensor(out=dst, in0=psv, in1=gb, op=ALU.mult),
                             reads=[pk, "gffnc"], writes=["XT%d" % i])
                    else:
                        for j in range(4):
                            kc = g4 * 4 + j
                            S.op("act", lambda E, j=j, kc=kc, ps=ps: E.activation(out=XT[i][:, kc, :], in_=ps[:, j * 128:(j + 1) * 128], func=AF.Copy,
                                                                                scale=gffnc[:, kc:kc + 1]),
                                 reads=[pk, "gffnc"], writes=["XT%d" % i])

            def b_gateup(e):
                i = e % 2
                for which, Wt, wk in ((0, WG[i], "WG%d" % i), (1, WU[i], "WU%d" % i)):
                    for kc in range(16):
                        S.op("pe", lambda E, which=which, Wt=Wt, kc=kc: E.matmul(PS[2][:, which * 256:(which + 1) * 256], lhsT=XT[i][:, kc, :],
                                                                               rhs=Wt[:, kc, :], start=(kc == 0), stop=(kc == 15)),
                             reads=["XT%d" % i, wk], writes=["p2"], signal=(kc == 15))
                rstd = small[:, 46 + i:47 + i]
                S.op("act", lambda E: E.activation(out=SG[:], in_=PS[2][:, 0:256], func=AF.Silu, scale=rstd),
                     reads=["p2", "brstd%d" % i], writes=["SG"])
                S.op("dve", lambda E: E.scalar_tensor_tensor(out=Hh[:], in0=PS[2][:, 256:512], scalar=rstd, in1=SG[:], op0=ALU.mult, op1=ALU.mult),
                     reads=["p2", "brstd%d" % i, "SG"], writes=["Hh"])

            def b_down(e):
                i = e % 2
                for fc in range(2):
                    S.op("pe", lambda E, fc=fc: E.transpose(out=PS[3][:, fc * 128:(fc + 1) * 128], in_=Hh[:].rearrange("s (p c) -> s c p", c=2)[:, fc, :], identity=ident[:]),
                         reads=["Hh", "ident"], writes=["p3"], signal=(fc == 1))
                S.op("act", lambda E: E.activation(out=HT[:].rearrange("p a b -> p (a b)"), in_=PS[3][:, 0:256], func=AF.Copy),
                     reads=["p3"], writes=["HT"])
                for dg in range(4):
                    for fc in range(2):
                        S.op("pe", lambda E, dg=dg, fc=fc: E.matmul(PS[4 + dg][:], lhsT=HT[:, fc, :], rhs=WD[i][:, fc, dg * 512:(dg + 1) * 512],
                                                                   start=(fc == 0), stop=(fc == 1)),
                             reads=["HT", "WD%d" % i], writes=["p%d" % (4 + dg)], signal=(fc == 1))
                    dst = Yb[i][:, dg * 512:(dg + 1) * 512]
                    if dg % 2 == 0:
                        S.op("act", lambda E, dg=dg, dst=dst: E.activation(out=dst, in_=PS[4 + dg][:], func=AF.Copy),
                             reads=["p%d" % (4 + dg)], writes=["Yb%d" % i])
                    else:
                        S.op("dve", lambda E, dg=dg, dst=dst: E.tensor_copy(out=dst, in_=PS[4 + dg][:]), reads=["p%d" % (4 + dg)], writes=["Yb%d" % i])
                S.dma("pool", ys_r[e], Yb[i][:, :], reads=["Yb%d" % i], writes=["Ys"])

            S.dma("pool", Xg[0][:, :], h1_d[:, :], reads=["h1d", "tokE"], writes=["Xg0"], in_off=tokE[:, 0:1], bound="tok")
            S.dma("pool", WG[0][:], wg_r[0], writes=["WG0"])
            S.dma("pool", WU[0][:], wu_r[0], writes=["WU0"])
            b_load_d(0)
            if NE_RUN > 1:
                b_load_gu(1)
                b_load_d(1)
            b_transpose(0)
            for e in range(NE_RUN):
                b_gateup(e)
                if e + 1 < NE_RUN:
                    b_transpose(e + 1)
                if e + 2 < NE_RUN:
                    b_load_gu(e + 2)
                b_down(e)
                if e + 2 < NE_RUN:
                    b_load_d(e + 2)

        S.barrier()
        with contextlib.ExitStack() as ec:
            def sc(name, shape, dtype):
                return ec.enter_context(nc.sbuf_tensor("c_" + name, shape, dtype))
            NB2 = 3
            Y1 = [sc("Y1_%d" % i, [128, D], F32) for i in range(NB2)]
            Y2 = [sc("Y2_%d" % i, [128, D], F32) for i in range(NB2)]
            acc = [sc("acc%d" % i, [128, D], F32) for i in range(NB2)]
            stmp = sc("stmp", [128, 1024], F32)
            gfin = sc("gfin", [128, D], F32)
            S.dma("sp", gfin[:], gfin_d, writes=["gfin"])
            for i in range(NB2):
                S.op("dve", lambda E, i=i: E.memset(Y1[i][:], 0.0), writes=["Y1_%d" % i])
                S.op("dve", lambda E, i=i: E.memset(Y2[i][:], 0.0), writes=["Y2_%d" % i])
            def b2_load(sg):
                i = sg % NB2
                S.dma("pool", Y1[i][:, :], Ys_d[:, :], reads=["Ys", "ridx"], writes=["Y1_%d" % i], in_off=ridx[:, sg, 0:1])
                S.dma("pool", Y2[i][:, :], Ys_d[:, :], reads=["Ys", "ridx"], writes=["Y2_%d" % i], in_off=ridx[:, sg, 1:2])
                S.dma("sp", acc[i][:], h1_d[sg * 128:(sg + 1) * 128, :], reads=["h1d"], writes=["acc%d" % i])

            NSG = 2 * NT_RUN
            for sg in range(min(NB2 - 1, NSG)):
                b2_load(sg)
            for sg in range(NSG):
                i = sg % NB2
                if sg + NB2 - 1 < NSG:
                    b2_load(sg + NB2 - 1)
                av = acc[i][:]
                ak = "acc%d" % i
                S.op("dve", lambda E, av=av, i=i, sg=sg: E.scalar_tensor_tensor(out=av, in0=Y1[i][:], scalar=wts[:, sg, 0:1], in1=av,
                                                                                op0=ALU.mult, op1=ALU.add),
                     reads=["Y1_%d" % i, "wts", ak], writes=[ak])
                S.op("dve", lambda E, av=av, i=i, sg=sg: E.scalar_tensor_tensor(out=av, in0=Y2[i][:], scalar=wts[:, sg, 1:2], in1=av,
                                                                                op0=ALU.mult, op1=ALU.add),
                     reads=["Y2_%d" % i, "wts", ak], writes=[ak])
                S.op("act", lambda E, av=av: E.activation(out=stmp[:], in_=av[:, 0:1024], func=AF.Square, accum_out=small[:, 40:41]),
                     reads=[ak], writes=["stmp", "fss"])
                S.op("act", lambda E, av=av: E.activation(out=stmp[:], in_=av[:, 1024:2048], func=AF.Square, accum_out=small[:, 41:42]),
                     reads=[ak], writes=["stmp", "fss"])
                S.op("dve", lambda E: E.tensor_tensor(out=small[:, 42:43], in0=small[:, 40:41], in1=small[:, 41:42], op=ALU.add),
                     reads=["fss"], writes=["fss2"])
                pow_rstd(small[:, 43:44], small[:, 42:43], 1, 1.0 / D, ["fss2"], ["frstd"])
                S.op("dve", lambda E, av=av: E.scalar_tensor_tensor(out=av, in0=av, scalar=small[:, 43:44], in1=gfin[:],
                                                                   op0=ALU.mult, op1=ALU.mult),
                     reads=[ak, "frstd", "gfin"], writes=[ak])
                S.dma("sp", y[sg * 128:(sg + 1) * 128, :], av, reads=[ak], writes=["y"])

        S.finish()
        with nc.Block() as block:
            @block.tensor
            def _(E):
                S.emit("pe", E)

            @block.scalar
            def _(E):
                S.emit("act", E)

            @block.vector
            def _(E):
                S.emit("dve", E)

            @block.gpsimd
            def _(E):
                S.emit("pool", E)

            @block.sync
            def _(E):
                S.emit("sp", E)
    return nc


def _consts():
    ident = np.eye(128, dtype=np.float32)
    j = np.arange(128)[:, None]
    i = np.arange(128)[None, :]
    same = (j // 64) == (i // 64)
    low = same & (j <= i)
    tri = np.where(low, -1.0 / 16.0, 0.0).astype(np.float32)
    maskT = low.astype(np.float32)
    mask4 = np.tile(maskT, (1, 4)).astype(np.float32)
    q = np.arange(128)[:, None]
    k = np.arange(256)[None, :]
    rel = q + 128 - k
    valid = (rel >= 0) & (rel < 128)
    relm = np.where(valid, -rel.astype(np.float32), -1e9).astype(np.float32)
    relm0 = np.where(valid & (k >= 128), -rel.astype(np.float32), -1e9).astype(np.float32)
    rconst = np.zeros((128, 192), np.float32)
    e = np.arange(NE, dtype=np.float64)[None, :]
    p = np.arange(128, dtype=np.float64)[:, None]
    rconst[:, 0:NE] = e - NE
    triu = (np.arange(128)[:, None] <= np.arange(128)[None, :]).astype(np.float32)
    tokid = (np.arange(16, dtype=np.int32)[None, :] * 128 + np.arange(128, dtype=np.int32)[:, None]).astype(np.int32)
    tabinit = np.full((NSLOT, 1), int(BIG), np.int32)
    return ident, tri, mask4, np.concatenate([relm, relm0], axis=1).astype(np.float32), rconst, triu, tokid, tabinit


_NC_CACHE = {}
GROUP_C0_H = [c * 256 for c in range(12)] + [3088, 3344, 3600, 3856, 4112, 4368]


def kernel(x, norm_mix_g, w_in, w_gk_up, b_gk, gla_norm_g, swa_sinks, w_out, norm_ffn_g, w_group, b_group,
           w_router, b_router, w_gate, w_up, w_down, norm_final_g):
    f = lambda a: np.ascontiguousarray(np.asarray(a), dtype=np.float32)
    x = f(x)
    ident, tri, mask4, relm, rconst, triu, tokid, tabinit = _consts()
    bc = lambda v: np.ascontiguousarray(np.broadcast_to(f(v).reshape(1, -1), (128, f(v).size)))
    common = {
        "w_in_g": np.ascontiguousarray(np.stack([f(w_in)[0][:, c:c + 256] for c in GROUP_C0_H])),
        "w_glr": np.ascontiguousarray(f(w_in)[0][:, 3072:3088]),
        "w_out_g": np.ascontiguousarray(np.stack([f(w_out)[0][:, c * 256:(c + 1) * 256] for c in range(8)])),
        "w_gate": f(w_gate)[0], "w_up": f(w_up)[0], "w_down": f(w_down)[0],
        "w_rt": np.ascontiguousarray(np.concatenate([f(w_group)[0], f(w_router)[0]], axis=1)),
        "g_mix": f(norm_mix_g)[0], "g_ffn": f(norm_ffn_g)[0],
        "gfin_bc": bc(norm_final_g), "gnorm_bc": bc(f(gla_norm_g)[0]), "sinks_bc": bc(f(swa_sinks)[0]),
        "bcat_bc": bc(np.concatenate([f(b_group)[0], f(b_router)[0]])),
        "wgk_aug": np.ascontiguousarray(np.concatenate([f(w_gk_up)[0], f(b_gk)[0][None, :]], axis=0)),
        "ident": ident, "tri": tri, "mask4": mask4, "relm": relm, "rconst": rconst, "triu": triu, "tokid": tokid, "tabinit": tabinit,
    }
    if "nc" not in _NC_CACHE:
        _NC_CACHE["nc"] = build_nc()
    nc = _NC_CACHE["nc"]
    in_maps = [dict(common, x=np.ascontiguousarray(x[b])) for b in range(8)]
    res = run_bass_kernel_spmd(nc, in_maps, core_ids=list(range(8)))
    return np.stack([np.asarray(r["y"], dtype=np.float32) for r in res.results], axis=0)
```
